# Optimizing a Trainium2 kernel written in Bass

```python
import jax, jax.numpy as jnp
from jax import lax
import numpy as np

D_MODEL = 4096
BATCH = 2
SEQ = 8192
DEPTH = 2

DEEPNORM_ALPHA = (2.0 * DEPTH) ** 0.25
DEEPNORM_BETA = (8.0 * DEPTH) ** -0.25
LN_EPS = 1e-5
RMS_EPS = 1e-5

POOL_WINDOWS = (2, 4, 8, 16)
POOL_GROUPS = 4
D_POOL = D_MODEL // 2
POOL_GROUP_DIM = D_POOL // POOL_GROUPS

SSD_HEADDIM = 64
D_SSD = (3 * D_MODEL) // 2
SSD_HEADS = D_SSD // SSD_HEADDIM
SSD_GROUPS = 8
SSD_STATE = 128
SSD_CONV = 4
SSD_CHUNK = 256
D_SSD_CONV = D_SSD + 2 * SSD_GROUPS * SSD_STATE
L0_IN = D_POOL + D_SSD + D_SSD_CONV + SSD_HEADS
L0_MIX = D_POOL + D_SSD

D_CONV = D_MODEL // 2
CONV_WIDTH = 3

SB_HEADS = 16
SB_HEADDIM = 128
D_SB = SB_HEADS * SB_HEADDIM
SB_BLOCK = 128
L1_IN = 3 * D_CONV + 3 * D_SB
L1_MIX = D_CONV + D_SB

N_EXPERTS = 32
TOP_K = 4
D_EXPERT = 512
SWIGLU_LIMIT = 7.0
SWIGLU_ALPHA = 1.702

kernel_name = "pool_ssd_shortconv_stickbreak_moe_deepnorm"


def layer_norm(x, g, b):
    xf = x.astype(jnp.float32)
    mu = jnp.mean(xf, axis=-1, keepdims=True)
    xc = xf - mu
    var = jnp.mean(xc * xc, axis=-1, keepdims=True)
    return (xc * lax.rsqrt(var + LN_EPS) * g + b).astype(x.dtype)


def causal_depthwise_conv(x, w):
    k, c = w.shape
    return lax.conv_general_dilated(
        x, w[:, None, :].astype(x.dtype), window_strides=(1,), padding=[(k - 1, 0)],
        dimension_numbers=("NWC", "WIO", "NWC"), feature_group_count=c)


def pool_mixer(u, pool_w, pool_scale):
    b, s, _ = u.shape
    ug = u.reshape(b, s, POOL_GROUPS, POOL_GROUP_DIM)
    cs = jnp.cumsum(ug.astype(jnp.float32), axis=1)
    pos = jnp.arange(1, s + 1, dtype=jnp.float32)
    outs = []
    for g, w in enumerate(POOL_WINDOWS):
        c = cs[:, :, g]
        prev = jnp.pad(c, ((0, 0), (w, 0), (0, 0)))[:, :s]
        mean = (c - prev) / jnp.minimum(pos, float(w))[None, :, None]
        outs.append(mean.astype(u.dtype) - ug[:, :, g])
    m = jnp.stack(outs, axis=2)
    y = jnp.einsum("bsgc,gcd->bsgd", m, pool_w)
    return y.reshape(b, s, D_POOL) * pool_scale


def ssd_chunked_scan(xh, dt, a, bmat, cmat):
    b, s, h, p = xh.shape
    g, n = bmat.shape[2], bmat.shape[3]
    e = h // g
    pad = (-s) % SSD_CHUNK
    if pad:
        padw = lambda t: jnp.pad(t, [(0, 0), (0, pad)] + [(0, 0)] * (t.ndim - 2))
        xh, dt, bmat, cmat = padw(xh), padw(dt), padw(bmat), padw(cmat)
    nc = (s + pad) // SSD_CHUNK
    ln = SSD_CHUNK
    xdt = (xh.astype(jnp.float32) * dt[..., None]).reshape(b, nc, ln, g, e, p)
    da = (dt * a).reshape(b, nc, ln, g, e)
    bc = bmat.reshape(b, nc, ln, g, n)
    cc = cmat.reshape(b, nc, ln, g, n)
    xdt, da, bc, cc = (jnp.moveaxis(t, 1, 0) for t in (xdt, da, bc, cc))
    causal = jnp.tril(jnp.ones((ln, ln), dtype=bool))[None, :, :, None, None]

    def step(state, inp):
        xc, dac, bcc, ccc = inp
        acum = jnp.cumsum(dac, axis=1)
        seg = acum[:, :, None] - acum[:, None, :]
        decay = jnp.exp(jnp.where(causal, seg, -jnp.inf))
        cb = jnp.einsum("btgn,bsgn->btsg", ccc, bcc)
        y_diag = jnp.einsum("btsg,btsge,bsgep->btgep", cb, decay, xc)
        y_off = jnp.einsum("btgn,bgepn->btgep", ccc, state) * jnp.exp(acum)[..., None]
        to_end = jnp.exp(acum[:, -1:] - acum)
        new_state = (state * jnp.exp(acum[:, -1])[..., None, None]
                     + jnp.einsum("bsgn,bsge,bsgep->bgepn", bcc, to_end, xc))
        return new_state, y_diag + y_off

    state0 = jnp.zeros((b, g, e, p, n), jnp.float32)
    _, y = lax.scan(step, state0, (xdt, da, bc, cc))
    return jnp.moveaxis(y, 0, 1).reshape(b, nc * ln, h, p)[:, :s]


def ssd_mixer(z, xbc, dt_raw, conv_w, conv_b, dt_bias, a_log, d_skip, norm_w):
    b, s, _ = z.shape
    xbc = jax.nn.silu(causal_depthwise_conv(xbc, conv_w) + conv_b)
    gn = SSD_GROUPS * SSD_STATE
    xs, bm, cm = jnp.split(xbc, [D_SSD, D_SSD + gn], axis=-1)
    xh = xs.reshape(b, s, SSD_HEADS, SSD_HEADDIM)
    dt = jax.nn.softplus(dt_raw.astype(jnp.float32) + dt_bias.astype(jnp.float32))
    a = -jnp.exp(a_log.astype(jnp.float32))
    y = ssd_chunked_scan(xh, dt, a, bm.reshape(b, s, SSD_GROUPS, SSD_STATE),
                         cm.reshape(b, s, SSD_GROUPS, SSD_STATE))
    y = y + d_skip.astype(jnp.float32)[:, None] * xh.astype(jnp.float32)
    y = y.reshape(b, s, D_SSD) * jax.nn.silu(z.astype(jnp.float32))
    yg = y.reshape(b, s, SSD_GROUPS, D_SSD // SSD_GROUPS)
    yg = yg * lax.rsqrt(jnp.mean(yg * yg, axis=-1, keepdims=True) + RMS_EPS)
    return (yg.reshape(b, s, D_SSD) * norm_w).astype(z.dtype)


def pool_ssd_mixer(x, w_in, conv_w, conv_b, dt_bias, a_log, d_skip, ssm_norm_w,
                   pool_w, pool_scale, w_out):
    h = x @ w_in
    u, z, xbc, dt_raw = jnp.split(h, [D_POOL, D_POOL + D_SSD, D_POOL + D_SSD + D_SSD_CONV], axis=-1)
    y_pool = pool_mixer(u, pool_w, pool_scale)
    y_ssd = ssd_mixer(z, xbc, dt_raw, conv_w, conv_b, dt_bias, a_log, d_skip, ssm_norm_w)
    return jnp.concatenate([y_pool, y_ssd], axis=-1) @ w_out


def stick_breaking_attention(q, k, v):
    b, s, h, dh = q.shape
    nb = s // SB_BLOCK
    scale = dh ** -0.5
    qb = q.reshape(b, nb, SB_BLOCK, h, dh).transpose(1, 0, 3, 2, 4)
    kpos = jnp.arange(s)

    def block(inp):
        qi, i = inp
        zs = jnp.einsum("bhqd,bkhd->bhqk", qi, k).astype(jnp.float32) * scale
        qpos = i * SB_BLOCK + jnp.arange(SB_BLOCK)
        mask = kpos[None, :] < qpos[:, None]
        log_stay = jnp.where(mask, -jax.nn.softplus(zs), 0.0)
        after = lax.cumsum(log_stay, axis=3, reverse=True) - log_stay
        w = jnp.where(mask, jnp.exp(jax.nn.log_sigmoid(zs) + after), 0.0)
        return jnp.einsum("bhqk,bkhd->bqhd", w.astype(v.dtype), v)

    o = lax.map(block, (qb, jnp.arange(nb)))
    return o.transpose(1, 0, 2, 3, 4).reshape(b, s, h * dh)


def shortconv_sb_mixer(x, w_in, conv_w, w_out):
    b, s, _ = x.shape
    h = x @ w_in
    c0 = D_CONV
    bg, cg, xin, q, k, v = jnp.split(
        h, [c0, 2 * c0, 3 * c0, 3 * c0 + D_SB, 3 * c0 + 2 * D_SB], axis=-1)
    y_conv = bg * causal_depthwise_conv(cg * xin, conv_w)
    hs = (b, s, SB_HEADS, SB_HEADDIM)
    y_sb = stick_breaking_attention(q.reshape(hs), k.reshape(hs), v.reshape(hs))
    return jnp.concatenate([y_conv, y_sb], axis=-1) @ w_out


def moe_ffn(x, router_w, router_b, w1, b1, w2, b2):
    b, s, d = x.shape
    t = x.reshape(b * s, d)
    logits = (t @ router_w + router_b).astype(jnp.float32)
    top_val, top_idx = lax.top_k(logits, TOP_K)
    top_w = jax.nn.softmax(top_val, axis=-1)
    gate = jnp.einsum("tk,tke->te", top_w, jax.nn.one_hot(top_idx, N_EXPERTS, dtype=jnp.float32))

    def expert(acc, inp):
        w1e, b1e, w2e, b2e, ge = inp
        hh = t @ w1e + b1e
        glu = jnp.minimum(hh[:, :D_EXPERT], SWIGLU_LIMIT)
        lin = jnp.clip(hh[:, D_EXPERT:], -SWIGLU_LIMIT, SWIGLU_LIMIT)
        act = glu * jax.nn.sigmoid(SWIGLU_ALPHA * glu) * (lin + 1.0)
        out = act @ w2e + b2e
        return acc + ge[:, None] * out.astype(jnp.float32), None

    acc0 = jnp.zeros((b * s, d), jnp.float32)
    acc, _ = lax.scan(expert, acc0, (w1, b1, w2, b2, gate.T))
    return acc.reshape(b, s, d).astype(x.dtype)


def setup_inputs(seed: int = 0) -> dict:
    key = jax.random.key(seed)
    ks = iter(jax.random.split(key, 64))
    f32 = jnp.float32

    def nrm(shape, scale):
        return jax.random.normal(next(ks), shape, f32) * scale

    def unif(shape, lo, hi):
        return jax.random.uniform(next(ks), shape, f32, lo, hi)

    d = D_MODEL
    out = {"x": nrm((BATCH, SEQ, d), 1.0)}
    dt0 = jnp.exp(unif((SSD_HEADS,), float(np.log(1e-3)), float(np.log(1e-1))))
    out["l0_w_in"] = nrm((d, L0_IN), d ** -0.5)
    out["l0_conv_w"] = nrm((SSD_CONV, D_SSD_CONV), SSD_CONV ** -0.5)
    out["l0_conv_b"] = nrm((D_SSD_CONV,), 0.02)
    out["l0_dt_bias"] = dt0 + jnp.log(-jnp.expm1(-dt0))
    out["l0_a_log"] = jnp.log(unif((SSD_HEADS,), 1.0, 16.0))
    out["l0_d_skip"] = 1.0 + nrm((SSD_HEADS,), 0.1)
    out["l0_ssm_norm_w"] = 1.0 + nrm((D_SSD,), 0.02)
    out["l0_pool_w"] = nrm((POOL_GROUPS, POOL_GROUP_DIM, POOL_GROUP_DIM), POOL_GROUP_DIM ** -0.5)
    out["l0_pool_scale"] = 1.0 + nrm((D_POOL,), 0.02)
    out["l0_w_out"] = nrm((L0_MIX, d), DEEPNORM_BETA * L0_MIX ** -0.5)
    out["l0_ln_mix_g"] = 1.0 + nrm((d,), 0.02)
    out["l0_ln_mix_b"] = nrm((d,), 0.02)
    out["l0_router_w"] = nrm((d, N_EXPERTS), d ** -0.5)
    out["l0_router_b"] = nrm((N_EXPERTS,), 0.01)
    out["l0_w1"] = nrm((N_EXPERTS, d, 2 * D_EXPERT), d ** -0.5)
    out["l0_b1"] = nrm((N_EXPERTS, 2 * D_EXPERT), 0.01)
    out["l0_w2"] = nrm((N_EXPERTS, D_EXPERT, d), DEEPNORM_BETA * D_EXPERT ** -0.5)
    out["l0_b2"] = nrm((N_EXPERTS, d), 0.01)
    out["l0_ln_ffn_g"] = 1.0 + nrm((d,), 0.02)
    out["l0_ln_ffn_b"] = nrm((d,), 0.02)
    out["l1_w_in"] = nrm((d, L1_IN), d ** -0.5)
    out["l1_conv_w"] = nrm((CONV_WIDTH, D_CONV), CONV_WIDTH ** -0.5)
    out["l1_w_out"] = nrm((L1_MIX, d), DEEPNORM_BETA * L1_MIX ** -0.5)
    out["l1_ln_mix_g"] = 1.0 + nrm((d,), 0.02)
    out["l1_ln_mix_b"] = nrm((d,), 0.02)
    out["l1_router_w"] = nrm((d, N_EXPERTS), d ** -0.5)
    out["l1_router_b"] = nrm((N_EXPERTS,), 0.01)
    out["l1_w1"] = nrm((N_EXPERTS, d, 2 * D_EXPERT), d ** -0.5)
    out["l1_b1"] = nrm((N_EXPERTS, 2 * D_EXPERT), 0.01)
    out["l1_w2"] = nrm((N_EXPERTS, D_EXPERT, d), DEEPNORM_BETA * D_EXPERT ** -0.5)
    out["l1_b2"] = nrm((N_EXPERTS, d), 0.01)
    out["l1_ln_ffn_g"] = 1.0 + nrm((d,), 0.02)
    out["l1_ln_ffn_b"] = nrm((d,), 0.02)
    return out


def reference(x,
              l0_w_in, l0_conv_w, l0_conv_b, l0_dt_bias, l0_a_log, l0_d_skip, l0_ssm_norm_w,
              l0_pool_w, l0_pool_scale, l0_w_out, l0_ln_mix_g, l0_ln_mix_b,
              l0_router_w, l0_router_b, l0_w1, l0_b1, l0_w2, l0_b2, l0_ln_ffn_g, l0_ln_ffn_b,
              l1_w_in, l1_conv_w, l1_w_out, l1_ln_mix_g, l1_ln_mix_b,
              l1_router_w, l1_router_b, l1_w1, l1_b1, l1_w2, l1_b2, l1_ln_ffn_g, l1_ln_ffn_b):
    mix_params = (
        (l0_w_in, l0_conv_w, l0_conv_b, l0_dt_bias, l0_a_log, l0_d_skip, l0_ssm_norm_w,
         l0_pool_w, l0_pool_scale, l0_w_out),
        (l1_w_in, l1_conv_w, l1_w_out),
    )
    ln_mix = ((l0_ln_mix_g, l0_ln_mix_b), (l1_ln_mix_g, l1_ln_mix_b))
    moe_params = ((l0_router_w, l0_router_b, l0_w1, l0_b1, l0_w2, l0_b2),
                  (l1_router_w, l1_router_b, l1_w1, l1_b1, l1_w2, l1_b2))
    ln_ffn = ((l0_ln_ffn_g, l0_ln_ffn_b), (l1_ln_ffn_g, l1_ln_ffn_b))
    for i in range(DEPTH):
        if i % 2 == 0:
            m = pool_ssd_mixer(x, *mix_params[i])
        else:
            m = shortconv_sb_mixer(x, *mix_params[i])
        x = layer_norm(DEEPNORM_ALPHA * x + m, *ln_mix[i])
        x = layer_norm(DEEPNORM_ALPHA * x + moe_ffn(x, *moe_params[i]), *ln_ffn[i])
    return x
```

```python
import numpy as np
from contextlib import ExitStack
import concourse.bass as bass
import concourse.mybir as mybir
from concourse.bass_utils import run_bass_kernel_spmd

F32 = mybir.dt.float32
BF16 = mybir.dt.bfloat16
AF = mybir.ActivationFunctionType
ALU = mybir.AluOpType
AX = mybir.AxisListType

D = 4096
KC = D // 128
NE = 32
TOPK = 4
DEXP = 512
ALPHA = 4.0 ** 0.25
LN_EPS = 1e-5
RMS_EPS = 1e-5
LIMIT = 7.0
SW_ALPHA = 1.702


class Chan:
    def __init__(self, prog, name, step):
        self.prog, self.name, self.step = prog, name, step
        self.ep = 30000 if step == 1 else 1800
        self.sems = []
        self.cnt = 0

    def sem_val(self, n):
        k = (n - 1) // self.ep
        while len(self.sems) <= k:
            self.sems.append(self.prog.new_sem(f"{self.name}_{len(self.sems)}"))
        return self.sems[k], ((n - 1) % self.ep + 1) * self.step


class Buf:
    def __init__(self, name, t=None, excl=False):
        self.name, self.t = name, t
        self.w = None
        self.r = {}
        self.excl = excl

    def __getitem__(self, k):
        return self.t[k]


class Prog:
    def __init__(self, nc, es, same_engine_sync=True):
        self.nc, self.es = nc, es
        self.E = {"pe": nc.tensor, "act": nc.scalar, "dve": nc.vector, "pool": nc.gpsimd, "sp": nc.sync}
        self.nsem = 0
        self.chan = {k: Chan(self, k, 1) for k in ("pe", "act", "dve", "pool")}
        self.seen = {k: {} for k in self.E}
        self.ses = same_engine_sync
        self.ntok = 0

    def new_sem(self, name):
        self.nsem += 1
        return self.es.enter_context(self.nc.semaphore(f"s_{name}_{self.nsem}"))

    def sbuf(self, name, shape, dt, es=None):
        return Buf(name, (es or self.es).enter_context(self.nc.sbuf_tensor(name, list(shape), dt)))

    def psum(self, name, shape, dt=F32):
        return Buf(name, self.es.enter_context(self.nc.psum_tensor(name, list(shape), dt)), excl=True)

    def token(self, name=None):
        self.ntok += 1
        return Chan(self, name or f"tok{self.ntok}", 16)

    def _wait(self, eng, deps):
        for ch, n in deps:
            if ch is self.chan.get(eng) and (eng == "pe" or not self.ses):
                continue
            if self.seen[eng].get(ch, 0) >= n:
                continue
            s, v = ch.sem_val(n)
            self.E[eng].wait_ge(s, v)
            self.seen[eng][ch] = n

    @staticmethod
    def _deps(reads, writes, own=None):
        deps = set()
        for b in reads:
            if b.w:
                deps.add(b.w)
            if b.excl:
                for ch, ev in b.r.items():
                    if ch is not own:
                        deps.add(ev)
        for b in writes:
            if b.w:
                deps.add(b.w)
            for ev in b.r.values():
                deps.add(ev)
        return deps

    def op(self, eng, reads, writes, fn):
        own = self.chan[eng]
        deps = self._deps(reads, writes, own)
        self._wait(eng, deps)
        inst = fn(self.E[eng])
        own.cnt += 1
        s, v = own.sem_val(own.cnt)
        inst.then_inc(s, 1)
        ev = (own, own.cnt)
        for b in reads:
            b.r[own] = ev
        for b in writes:
            b.w = ev
            b.r = {}
        return ev

    def dma(self, q, tok, out, in_, reads=(), writes=()):
        deps = self._deps(reads, writes)
        if tok.cnt:
            deps.add((tok, tok.cnt))
        self._wait(q, deps)
        inst = self.E[q].dma_start(out=out, in_=in_)
        tok.cnt += 1
        s, v = tok.sem_val(tok.cnt)
        inst.then_inc(s, 16)
        ev = (tok, tok.cnt)
        for b in reads:
            b.r[tok] = ev
        for b in writes:
            b.w = ev
            b.r = {}
        return ev

    def barrier(self, toks=()):
        evs = {(ch, ch.cnt) for ch in self.chan.values() if ch.cnt}
        evs |= {(t, t.cnt) for t in toks if t.cnt}
        for eng in self.E:
            self._wait(eng, evs)

    def finish(self, eng, toks):
        self._wait(eng, {(t, t.cnt) for t in toks if t.cnt})


def _mm(pe, out, lhsT, rhs, start, stop):
    return pe.matmul(out, lhsT, rhs, start=start, stop=stop)


def emit_layernorm(P, C, r, TT, g_ap, b_ap, ps_a, ps_b, sq, stat, x16=None):
    ones = C["ones_f"]
    for c in range(KC):
        P.op("pe", [r, ones], [ps_a], lambda e, c=c: _mm(e, ps_a[:, :TT], ones[:, :], r[:, c, :], c == 0, c == KC - 1))
    for c in range(KC):
        s = sq[c % 2]
        P.op("act", [r], [s], lambda e, c=c, s=s: e.activation(out=s[:, :], in_=r[:, c, :], func=AF.Square))
        P.op("pe", [s, ones], [ps_b], lambda e, c=c, s=s: _mm(e, ps_b[:, :TT], ones[:, :], s[:, :], c == 0, c == KC - 1))
    mean, var, rstd, nmr = (stat[:, i, :] for i in range(4))
    P.op("dve", [ps_a], [stat], lambda e: e.tensor_scalar(out=mean, in0=ps_a[:, :TT], scalar1=1.0 / D, scalar2=None, op0=ALU.mult))
    P.op("dve", [stat], [stat], lambda e: e.tensor_tensor(out=nmr, in0=mean, in1=mean, op=ALU.mult))
    P.op("dve", [ps_b, stat], [stat], lambda e: e.scalar_tensor_tensor(out=var, in0=ps_b[:, :TT], scalar=1.0 / D, in1=nmr, op0=ALU.mult, op1=ALU.subtract))
    P.op("dve", [stat], [stat], lambda e: e.tensor_scalar(out=var, in0=var, scalar1=LN_EPS, scalar2=None, op0=ALU.add))
    P.op("act", [stat], [stat], lambda e: e.activation(out=var, in_=var, func=AF.Ln))
    P.op("act", [stat], [stat], lambda e: e.activation(out=rstd, in_=var, func=AF.Exp, scale=-0.5))
    P.op("dve", [stat], [stat], lambda e: e.scalar_tensor_tensor(out=nmr, in0=mean, scalar=-1.0, in1=rstd, op0=ALU.mult, op1=ALU.mult))
    for c in range(KC):
        s = sq[c % 2]
        P.op("dve", [r, stat], [s], lambda e, c=c, s=s: e.tensor_tensor(out=s[:, :], in0=r[:, c, :], in1=rstd, op=ALU.mult))
        P.op("dve", [s, stat], [s], lambda e, s=s: e.tensor_tensor(out=s[:, :], in0=s[:, :], in1=nmr, op=ALU.add))
        P.op("act", [s, C["lnp"]], [r], lambda e, c=c, s=s: e.activation(out=r[:, c, :], in_=s[:, :], func=AF.Identity, scale=g_ap(c), bias=b_ap(c)))
        if x16 is not None:
            P.op("pool", [r], [x16], lambda e, c=c: e.tensor_copy(out=x16[:, c, :], in_=r[:, c, :]))


def build_F(NT, KM, TT=512):
    KMC = KM // 128
    NTT = NT // TT
    NSUB = TT // 128
    NH1 = max(1, KMC // 32)
    nc = bass.Bass("TRN2", target_bir_lowering=False)
    dr = lambda n, s, d=F32, k="ExternalInput": nc.dram_tensor(n, list(s), d, kind=k).ap()
    ymix = dr("ymix", [KM, NT], BF16)
    xres = dr("xres", [D, NT])
    w_out = dr("w_out", [KM, D])
    lnp_d = dr("lnp", [128, 4 * KC])
    rw_d = dr("rw", [128, KC * NE])
    rb_d = dr("rb", [128, NE])
    w1 = dr("w1", [NE, D, 2 * DEXP])
    b1_d = dr("b1t", [128, NE * 8])
    w2 = dr("w2", [NE, DEXP, D])
    b2_d = dr("b2", [NE, D])
    ident_d = dr("ident", [128, 128])
    out = dr("out", [D, NT], F32, "ExternalOutput")

    with ExitStack() as es:
        P = Prog(nc, es)
        r = P.sbuf("r", [128, KC, TT], F32)
        big = P.sbuf("big", [128, 32, TT], BF16)
        x16 = P.sbuf("x16", [128, KC, TT], BF16)
        NW = 3
        wbt = [es.enter_context(nc.sbuf_tensor(f"wb{i}", [128, 8, 512], BF16)) for i in range(NW)]
        wbh = [[Buf(f"wb{i}a", wbt[i]), Buf(f"wb{i}b", wbt[i])] for i in range(NW)]
        wtok = [[P.token(f"wt{i}a"), P.token(f"wt{i}b")] for i in range(NW)]
        sq = [P.sbuf(f"sq{i}", [128, TT], F32) for i in range(2)]
        stat = P.sbuf("stat", [128, 4, TT], F32)
        tg = [P.sbuf(f"tg{i}", [128, TT], F32) for i in range(2)]
        tl = [P.sbuf(f"tl{i}", [128, TT], F32) for i in range(2)]
        tsg = [P.sbuf(f"tsg{i}", [128, TT], F32) for i in range(2)]
        lnp = P.sbuf("lnp_s", [128, 4 * KC], F32)
        rw = P.sbuf("rw_s", [128, KC * NE], F32)
        rb = P.sbuf("rb_s", [128, NE], F32)
        b1t = P.sbuf("b1t_s", [128, NE * 8], F32)
        b2p = [P.sbuf(f"b2p{i}", [NE, 512], F32) for i in range(2)]
        b2tok = [P.token(f"b2t{i}") for i in range(2)]
        ident = P.sbuf("ident_s", [128, 128], F32)
        ones_f = P.sbuf("ones_f", [128, 128], F32)
        lg = P.sbuf("lg", [128, NSUB, NE], F32)
        gate = P.sbuf("gate", [128, NSUB, NE], F32)
        sm = P.sbuf("sm", [128, NSUB, 16], F32)
        gT = P.sbuf("gT", [NE, TT], F32)
        gTm = [P.sbuf(f"gTm{i}", [NE, TT], F32) for i in range(2)]
        ps = [P.psum(f"ps{i}", [128, 512]) for i in range(8)]
        C = {"ones_f": ones_f, "lnp": lnp}
        ctok = P.token("const")
        iotok = [P.token("io0"), P.token("io1"), P.token("io2")]
        otok = P.token("otok")

        for dst, src in ((lnp, lnp_d), (rw, rw_d), (rb, rb_d), (b1t, b1_d), (ident, ident_d)):
            P.dma("sp", ctok, dst[:, :], src[:, :], [], [dst])
        P.op("dve", [], [ones_f], lambda e: e.memset(ones_f[:, :], 1.0))

        ymix_v = ymix.rearrange("(c p) t -> p c t", p=128)
        xres_v = xres.rearrange("(c p) t -> p c t", p=128)
        out_v = out.rearrange("(c p) t -> p c t", p=128)
        wout_v = w_out.rearrange("(c p) d -> p c d", p=128)
        w1_v = w1.rearrange("e (c p) f -> p e c f", p=128)
        w2_v = w2.rearrange("e (c p) d -> p e c d", p=128)

        wi = [0]

        def wload(halves):
            i = wi[0] % NW
            wi[0] += 1
            for h, (dsl, src) in enumerate(halves):
                P.dma("pool", wtok[i][h], dsl(wbt[i]), src, [], [wbh[i][h]])
            return wbt[i], wbh[i]

        def run_stream(items, depth=2):
            loaded = []
            n = len(items)
            for i in range(min(depth, n)):
                loaded.append(wload(items[i][0]))
            for i in range(n):
                wt, wbufs = loaded[i]
                items[i][1](wt, wbufs)
                if i + depth < n:
                    loaded.append(wload(items[i + depth][0]))

        for tt in range(NTT):
            t0 = tt * TT
            tsl = slice(t0, t0 + TT)
            P.dma("sp", iotok[2], r[:, :, :], xres_v[:, :, tsl], [], [r])
            for hf in range(NH1):
                kc0 = hf * 32
                nk = min(32, KMC)
                for h in range(2):
                    cs = slice(h * nk // 2, (h + 1) * nk // 2)
                    P.dma("sp", iotok[h], big[:, cs, :], ymix_v[:, kc0 + cs.start:kc0 + cs.stop, tsl], [], [big])
                items = []
                for g in range(8):
                    dsl = slice(g * 512, (g + 1) * 512)
                    for kg in range(nk // 8):
                        k0 = kc0 + kg * 8
                        halves = [(lambda b: b[:, 0:4, :], wout_v[:, k0:k0 + 4, dsl]),
                                  (lambda b: b[:, 4:8, :], wout_v[:, k0 + 4:k0 + 8, dsl])]

                        def comp(wt, wbufs, g=g, kg=kg, hf=hf):
                            for kc in range(8):
                                k = kg * 8 + kc
                                for j in range(4):
                                    P.op("pe", wbufs + [big], [ps[j]], lambda e, kc=kc, j=j, k=k: _mm(
                                        e, ps[j][:, :TT], wt[:, kc, j * 128:(j + 1) * 128], big[:, k, :], k == 0, k == nk - 1))
                            if kg == nk // 8 - 1:
                                for j in range(4):
                                    c = g * 4 + j
                                    if hf == 0:
                                        P.op("dve", [r, ps[j]], [r], lambda e, c=c, j=j: e.scalar_tensor_tensor(
                                            out=r[:, c, :], in0=r[:, c, :], scalar=ALPHA, in1=ps[j][:, :TT], op0=ALU.mult, op1=ALU.add))
                                    else:
                                        P.op("dve", [r, ps[j]], [r], lambda e, c=c, j=j: e.tensor_tensor(
                                            out=r[:, c, :], in0=r[:, c, :], in1=ps[j][:, :TT], op=ALU.add))
                        items.append((halves, comp))
                run_stream(items)
            emit_layernorm(P, C, r, TT, lambda c: lnp[:, c:c + 1], lambda c: lnp[:, KC + c:KC + c + 1],
                           ps[4], ps[5], sq, stat, x16)
            for s in range(NSUB):
                for c in range(KC):
                    P.op("pe", [r, rw], [ps[6]], lambda e, s=s, c=c: _mm(
                        e, ps[6][:, s * NE:(s + 1) * NE], r[:, c, s * 128:(s + 1) * 128], rw[:, c * NE:(c + 1) * NE], c == 0, c == KC - 1))
            for s in range(NSUB):
                P.op("dve", [ps[6], rb], [lg], lambda e, s=s: e.tensor_tensor(out=lg[:, s, :], in0=ps[6][:, s * NE:(s + 1) * NE], in1=rb[:, :], op=ALU.add))
                P.op("dve", [lg], [sm], lambda e, s=s: e.max(out=sm[:, s, 0:8], in_=lg[:, s, :]))
                P.op("dve", [sm], [sm], lambda e, s=s: e.tensor_scalar(out=sm[:, s, 8:9], in0=sm[:, s, 0:1], scalar1=-1.0, scalar2=None, op0=ALU.mult))
                P.op("dve", [lg, sm], [gate], lambda e, s=s: e.tensor_scalar(out=gate[:, s, :], in0=lg[:, s, :], scalar1=sm[:, s, 3:4], scalar2=None, op0=ALU.is_ge))
                P.op("act", [lg, sm], [lg], lambda e, s=s: e.activation(out=lg[:, s, :], in_=lg[:, s, :], func=AF.Exp, bias=sm[:, s, 8:9], scale=1.0))
                P.op("dve", [lg, gate], [gate], lambda e, s=s: e.tensor_tensor(out=gate[:, s, :], in0=gate[:, s, :], in1=lg[:, s, :], op=ALU.mult))
                P.op("dve", [gate], [sm], lambda e, s=s: e.reduce_sum(out=sm[:, s, 9:10], in_=gate[:, s, :], axis=AX.X))
                P.op("dve", [sm], [sm], lambda e, s=s: e.reciprocal(out=sm[:, s, 10:11], in_=sm[:, s, 9:10]))
                P.op("dve", [gate, sm], [gate], lambda e, s=s: e.tensor_scalar(out=gate[:, s, :], in0=gate[:, s, :], scalar1=sm[:, s, 10:11], scalar2=None, op0=ALU.mult))
                P.op("pe", [gate, ident], [ps[7]], lambda e, s=s: e.transpose(ps[7][0:NE, s * 128:(s + 1) * 128], gate[:, s, :], ident[:, :]))
            P.op("act", [ps[7]], [gT], lambda e: e.copy(out=gT[:, :], in_=ps[7][0:NE, :TT]))
            for ph in range(4):
                items = []
                for el in range(8):
                    ex = ph * 8 + el
                    for jp in range(2):
                        for kg in range(4):
                            ksl = slice(kg * 8, (kg + 1) * 8)
                            halves = [(lambda b: b[:, :, 0:256], w1_v[:, ex, ksl, jp * 256:(jp + 1) * 256]),
                                      (lambda b: b[:, :, 256:512], w1_v[:, ex, ksl, 512 + jp * 256:512 + (jp + 1) * 256])]

                            def comp(wt, wbufs, ex=ex, el=el, jp=jp, kg=kg):
                                if jp == 0 and kg == 0:
                                    gm = gTm[ex % 2]
                                    P.op("dve", [gT, ident], [gm], lambda e: e.tensor_scalar(
                                        out=gm[:, :], in0=gT[:, :], scalar1=ident[0:NE, ex:ex + 1], scalar2=None, op0=ALU.mult))
                                    P.op("pe", [ones_f, gm], [ps[6]], lambda e: _mm(e, ps[6][:, :TT], ones_f[0:NE, :], gm[:, :], True, True))
                                for kc in range(8):
                                    k = kg * 8 + kc
                                    for q in range(4):
                                        P.op("pe", wbufs + [x16], [ps[q]], lambda e, kc=kc, q=q, k=k: _mm(
                                            e, ps[q][:, :TT], wt[:, kc, q * 128:(q + 1) * 128], x16[:, k, :], k == 0, k == KC - 1))
                                if kg == 3:
                                    for jj in range(2):
                                        j = jp * 2 + jj
                                        pg, pl = ps[jj], ps[2 + jj]
                                        a, l_, sg = tg[jj], tl[jj], tsg[jj]
                                        bg = b1t[:, ex * 8 + j:ex * 8 + j + 1]
                                        bl = b1t[:, ex * 8 + 4 + j:ex * 8 + 4 + j + 1]
                                        P.op("dve", [pg, b1t], [a], lambda e: e.tensor_scalar(out=a[:, :], in0=pg[:, :TT], scalar1=bg, scalar2=LIMIT, op0=ALU.add, op1=ALU.min))
                                        P.op("act", [a], [sg], lambda e: e.activation(out=sg[:, :], in_=a[:, :], func=AF.Sigmoid, scale=SW_ALPHA))
                                        P.op("dve", [pl, b1t], [l_], lambda e: e.tensor_scalar(out=l_[:, :], in0=pl[:, :TT], scalar1=bl, scalar2=LIMIT, op0=ALU.add, op1=ALU.min))
                                        P.op("dve", [l_], [l_], lambda e: e.tensor_scalar(out=l_[:, :], in0=l_[:, :], scalar1=-LIMIT, scalar2=1.0, op0=ALU.max, op1=ALU.add))
                                        P.op("dve", [a, sg], [a], lambda e: e.tensor_tensor(out=a[:, :], in0=a[:, :], in1=sg[:, :], op=ALU.mult))
                                        P.op("dve", [a, l_], [a], lambda e: e.tensor_tensor(out=a[:, :], in0=a[:, :], in1=l_[:, :], op=ALU.mult))
                                        P.op("dve", [a, ps[6]], [big], lambda e: e.tensor_tensor(out=big[:, el * 4 + j, :], in0=a[:, :], in1=ps[6][:, :TT], op=ALU.mult))
                            items.append((halves, comp))
                for g in range(8):
                    dsl = slice(g * 512, (g + 1) * 512)
                    for pc in range(4):
                        e0 = ph * 8 + pc * 2
                        halves = [(lambda b: b[:, 0:4, :], w2_v[:, e0, :, dsl]), (lambda b: b[:, 4:8, :], w2_v[:, e0 + 1, :, dsl])]

                        def comp(wt, wbufs, g=g, pc=pc, ph=ph, dsl=dsl):
                            if ph == 0 and pc == 0:
                                bp = b2p[g % 2]
                                P.dma("sp", b2tok[g % 2], bp[:, :], b2_d[:, dsl], [], [bp])
                                for j in range(4):
                                    P.op("pe", [bp, gT], [ps[j]], lambda e, j=j: _mm(
                                        e, ps[j][:, :TT], bp[:, j * 128:(j + 1) * 128], gT[:, :], True, False))
                            for kc in range(8):
                                k = pc * 8 + kc
                                for j in range(4):
                                    P.op("pe", wbufs + [big], [ps[j]], lambda e, kc=kc, j=j, k=k: _mm(
                                        e, ps[j][:, :TT], wt[:, kc, j * 128:(j + 1) * 128], big[:, k, :], (k == 0 and ph > 0), k == 31))
                            if pc == 3:
                                for j in range(4):
                                    c = g * 4 + j
                                    if ph == 0:
                                        P.op("dve", [r, ps[j]], [r], lambda e, c=c, j=j: e.scalar_tensor_tensor(
                                            out=r[:, c, :], in0=r[:, c, :], scalar=ALPHA, in1=ps[j][:, :TT], op0=ALU.mult, op1=ALU.add))
                                    else:
                                        P.op("dve", [r, ps[j]], [r], lambda e, c=c, j=j: e.tensor_tensor(
                                            out=r[:, c, :], in0=r[:, c, :], in1=ps[j][:, :TT], op=ALU.add))
                        items.append((halves, comp))
                run_stream(items)
            emit_layernorm(P, C, r, TT, lambda c: lnp[:, 2 * KC + c:2 * KC + c + 1], lambda c: lnp[:, 3 * KC + c:3 * KC + c + 1],
                           ps[4], ps[5], sq, stat, None)
            P.dma("sp", otok, out_v[:, :, tsl], r[:, :, :], [r], [])
        P.finish("sp", [otok])
    return nc


def _fm(a):
    return np.ascontiguousarray(a.reshape(-1, 128).T)


def host_inputs_F(ymixT, xresT, w_out, g1, b1_, g2, b2_, router_w, router_b, w1, b1, w2, b2):
    lnp = np.concatenate([_fm(g1), _fm(b1_), _fm(g2), _fm(b2_)], axis=1).astype(np.float32)
    rw = np.ascontiguousarray(router_w.reshape(KC, 128, NE).transpose(1, 0, 2).reshape(128, KC * NE))
    rb = np.ascontiguousarray(np.broadcast_to(router_b[None, :], (128, NE)))
    b1t = np.ascontiguousarray(b1.reshape(NE, 8, 128).transpose(2, 0, 1).reshape(128, NE * 8))
    return {
        "ymix": ymixT, "xres": xresT, "w_out": w_out, "lnp": lnp, "rw": rw, "rb": rb,
        "w1": w1, "b1t": b1t, "w2": w2, "b2": b2,
        "ident": np.eye(128, dtype=np.float32),
    }


def build_C(S, NHEAD=4):
    TT = 512
    TP = 256
    NT = S // TT
    NP = S // TP
    NB = S // 128
    nc = bass.Bass("TRN2", target_bir_lowering=False)
    dr = lambda n, s, d=F32, k="ExternalInput": nc.dram_tensor(n, list(s), d, kind=k).ap()
    xT = dr("xT", [D, S])
    wc = dr("wc", [D, 1536])
    wq = dr("wq", [D, 1536])
    cw_d = dr("convw", [128, 12])
    mask_d = dr("mask", [128, 4 * 512])
    tri_d = dr("tri", [128, 256], BF16)
    ymix = dr("ymix", [1024, S], BF16, "ExternalOutput")

    with ExitStack() as es:
        P = Prog(nc, es)
        wres = P.sbuf("wres", [128, KC, 1536], BF16)
        xb_t = [es.enter_context(nc.sbuf_tensor(f"xb{i}", [128, KC, TP], BF16)) for i in range(2)]
        xb = [[Buf(f"xb{i}_{h}", xb_t[i]) for h in range(4)] for i in range(2)]
        xtok = [[P.token(f"xt{i}_{h}") for h in range(4)] for i in range(2)]
        cw = P.sbuf("cw", [128, 12], F32)
        mask = P.sbuf("mask_s", [128, 4, 512], F32)
        tri = P.sbuf("tri_s", [128, 256], BF16)
        ps = [P.psum(f"ps{i}", [128, 512]) for i in range(8)]
        ctok = P.token("const")
        wtok = [P.token(f"wtk{i}") for i in range(6)]
        otok = [P.token("o0"), P.token("o1")]
        P.dma("sp", ctok, cw[:, :], cw_d[:, :], [], [cw])
        P.dma("sp", ctok, mask[:, :, :], mask_d.rearrange("p (i t) -> p i t", i=4), [], [mask])
        P.dma("sp", ctok, tri[:, :], tri_d[:, :], [], [tri])
        xT_v = xT.rearrange("(c p) t -> p c t", p=128)
        wc_v = wc.rearrange("(c p) f -> p c f", p=128)
        wq_v = wq.rearrange("(c p) f -> p c f", p=128)
        xi = [0]

        def xload(tt):
            i = xi[0] % 2
            xi[0] += 1
            for h in range(4):
                P.dma("pool", xtok[i][h], xb_t[i][:, h * 8:(h + 1) * 8, :], xT_v[:, h * 8:(h + 1) * 8, tt * TP:(tt + 1) * TP], [], [xb[i][h]])
            return xb_t[i], xb[i]

        for q in range(6):
            P.dma("pool", wtok[q], wres[:, :, q * 256:(q + 1) * 256], wc_v[:, :, q * 256:(q + 1) * 256], [], [wres])
        pbuf = P.sbuf("pbuf", [128, 4, TP + 2], F32)
        cgs = [P.sbuf(f"cgs{i}", [128, TP], F32) for i in range(2)]
        acc = [P.sbuf(f"cacc{i}", [128, TP], F32) for i in range(2)]
        yt = [P.sbuf(f"yt{i}", [128, 4, TP], BF16) for i in range(2)]
        P.op("dve", [], [pbuf], lambda e: e.memset(pbuf[:, :, :], 0.0))
        nxt = xload(0)
        for tt in range(NP):
            xt_, xbufs = nxt
            if tt + 1 < NP:
                nxt = xload(tt + 1)
            y = yt[tt % 2]
            for c in range(4):
                pa, pb, pc = ps[(c % 2) * 3], ps[(c % 2) * 3 + 1], ps[(c % 2) * 3 + 2]
                for (pp, col) in ((pa, 512 + c * 128), (pb, 1024 + c * 128), (pc, c * 128)):
                    for k in range(KC):
                        P.op("pe", xbufs + [wres], [pp], lambda e, pp=pp, col=col, k=k: _mm(
                            e, pp[:, :TP], wres[:, k, col:col + 128], xt_[:, k, :], k == 0, k == KC - 1))
                g_, a_ = cgs[c % 2], acc[c % 2]
                P.op("act", [pa], [g_], lambda e, g_=g_, pa=pa: e.copy(out=g_[:, :], in_=pa[:, :TP]))
                P.op("dve", [g_, pb], [pbuf], lambda e, g_=g_, pb=pb, c=c: e.tensor_tensor(out=pbuf[:, c, 2:TP + 2], in0=g_[:, :], in1=pb[:, :TP], op=ALU.mult))
                P.op("dve", [pbuf, cw], [a_], lambda e, a_=a_, c=c: e.tensor_scalar(out=a_[:, :], in0=pbuf[:, c, 0:TP], scalar1=cw[:, c:c + 1], scalar2=None, op0=ALU.mult))
                P.op("dve", [pbuf, cw, a_], [a_], lambda e, a_=a_, c=c: e.scalar_tensor_tensor(out=a_[:, :], in0=pbuf[:, c, 1:TP + 1], scalar=cw[:, 4 + c:5 + c], in1=a_[:, :], op0=ALU.mult, op1=ALU.add))
                P.op("dve", [pbuf, cw, a_], [a_], lambda e, a_=a_, c=c: e.scalar_tensor_tensor(out=a_[:, :], in0=pbuf[:, c, 2:TP + 2], scalar=cw[:, 8 + c:9 + c], in1=a_[:, :], op0=ALU.mult, op1=ALU.add))
                P.op("dve", [a_, pc], [y], lambda e, a_=a_, pc=pc, c=c, y=y: e.tensor_tensor(out=y[:, c, :], in0=a_[:, :], in1=pc[:, :TP], op=ALU.mult))
                P.op("dve", [pbuf], [pbuf], lambda e, c=c: e.tensor_copy(out=pbuf[:, c, 0:2], in_=pbuf[:, c, TP:TP + 2]))
            P.dma("sp", otok[tt % 2], ymix[0:512, tt * TP:(tt + 1) * TP].rearrange("(c p) t -> p c t", p=128), y[:, :, :], [y], [])

        P.barrier(wtok + otok + [t for tl_ in xtok for t in tl_])
        wres_t = wres.t
        wres = Buf("wres2", wres_t[:, 0:8, :].rearrange("p k (a f) -> p (k a) f", f=384))
        flat = lambda lo, hi: wres_t[:, lo:hi, :].rearrange("p k f -> p (k f)")
        QT = Buf("QT", flat(8, 14)[:, 0:S])
        KT = Buf("KT", flat(14, 20)[:, 0:S])
        V = Buf("V", flat(20, 26)[:, 0:NB * 128].rearrange("p (b d) -> p b d", d=128))
        eb = [P.sbuf(f"eb{i}", [128, TT], F32) for i in range(2)]
        spb = [P.sbuf(f"spb{i}", [128, TT], BF16) for i in range(2)]
        exb = [P.sbuf(f"exb{i}", [128, TT], F32) for i in range(2)]
        wbf = [P.sbuf(f"wbf{i}", [128, TT], BF16) for i in range(2)]
        yo = [P.sbuf(f"yo{i}", [128, TT], BF16) for i in range(2)]
        pz = [ps[0], ps[1]]
        pacc, po = ps[2], ps[3]
        scale = 128.0 ** -0.5
        for h in range(NHEAD):
            for q in range(3):
                P.dma("pool", wtok[q], wres[:, :, q * 128:(q + 1) * 128], wq_v[:, :, q * 512 + h * 128:q * 512 + (h + 1) * 128], [], [wres])
            nxt = xload(0)
            for tt in range(NP):
                xt_, xbufs = nxt
                if tt + 1 < NP:
                    nxt = xload(tt + 1)
                for (dst, col, pp) in ((QT, 0, ps[4]), (KT, 128, ps[5])):
                    for k in range(KC):
                        P.op("pe", xbufs + [wres], [pp], lambda e, pp=pp, col=col, k=k: _mm(
                            e, pp[:, :TP], wres[:, k, col:col + 128], xt_[:, k, :], k == 0, k == KC - 1))
                    P.op("act", [pp], [dst], lambda e, pp=pp, dst=dst, tt=tt: e.copy(out=dst[:, tt * TP:(tt + 1) * TP], in_=pp[:, :TP]))
                pv = ps[6 + tt % 2]
                for s in range(TP // 128):
                    for k in range(KC):
                        P.op("pe", xbufs + [wres], [pv], lambda e, pv=pv, s=s, k=k: _mm(
                            e, pv[:, s * 128:(s + 1) * 128], xt_[:, k, s * 128:(s + 1) * 128], wres[:, k, 256:384], k == 0, k == KC - 1))
                P.op("dve", [pv], [V], lambda e, pv=pv, tt=tt: e.tensor_copy(out=V[:, tt * (TP // 128):(tt + 1) * (TP // 128), :], in_=pv[:, :TP].rearrange("p (s d) -> p s d", d=128)))
            steps = [(g, kb) for g in range(NT) for kb in range(4 * g + 3, -1, -1)]

            def stageA(n):
                g, kb = steps[n]
                z, e_, sp_ = pz[n % 2], eb[n % 2], spb[n % 2]
                P.op("pe", [KT, QT], [z], lambda e: _mm(e, z[:, :], KT[:, kb * 128:(kb + 1) * 128], QT[:, g * TT:(g + 1) * TT], True, True))
                P.op("act", [z], [e_], lambda e: e.activation(out=e_[:, :], in_=z[:, :], func=AF.Exp, scale=scale))
                P.op("act", [e_], [sp_], lambda e: e.activation(out=sp_[:, :], in_=e_[:, :], func=AF.Ln, bias=1.0))
                i = kb - 4 * g
                if i >= 0:
                    P.op("pool", [sp_, mask], [sp_], lambda e: e.tensor_tensor(out=sp_[:, :], in0=sp_[:, :], in1=mask[:, i, :], op=ALU.mult))
                    P.op("pool", [e_, mask], [e_], lambda e: e.tensor_tensor(out=e_[:, :], in0=e_[:, :], in1=mask[:, i, :], op=ALU.mult))

            def stageB(n):
                g, kb = steps[n]
                e_, sp_, ex_, w_ = eb[n % 2], spb[n % 2], exb[n % 2], wbf[n % 2]
                first, last = kb == 4 * g + 3, kb == 0
                P.op("pe", [tri, sp_], [pacc], lambda e: _mm(e, pacc[:, :], tri[:, 0:128], sp_[:, :], first, False))
                P.op("act", [pacc], [ex_], lambda e: e.activation(out=ex_[:, :], in_=pacc[:, :], func=AF.Exp))
                P.op("pe", [tri, sp_], [pacc], lambda e: _mm(e, pacc[:, :], tri[:, 128:256], sp_[:, :], False, last))
                P.op("dve", [e_, ex_], [w_], lambda e: e.tensor_tensor(out=w_[:, :], in0=e_[:, :], in1=ex_[:, :], op=ALU.mult))
                P.op("pe", [V, w_], [po], lambda e: _mm(e, po[:, :], V[:, kb, :], w_[:, :], first, last))
                if last:
                    y = yo[g % 2]
                    P.op("act", [po], [y], lambda e: e.copy(out=y[:, :], in_=po[:, :]))
                    P.dma("sp", otok[g % 2], ymix[512 + h * 128:512 + (h + 1) * 128, g * TT:(g + 1) * TT], y[:, :], [y], [])

            stageA(0)
            for n in range(len(steps)):
                if n + 1 < len(steps):
                    stageA(n + 1)
                stageB(n)
        P.finish("sp", otok)
    return nc


def host_consts_C():
    import ml_dtypes
    s = np.arange(128)[:, None]
    t = np.arange(512)[None, :]
    mask = np.stack([(t > 128 * i + s).astype(np.float32) for i in range(4)], axis=1)
    j = np.arange(128)[:, None]
    s2 = np.arange(128)[None, :]
    tri = np.concatenate([-(j >= s2).astype(np.float32), -(j < s2).astype(np.float32)], axis=1)
    return {"mask": np.ascontiguousarray(mask.reshape(128, 2048)), "tri": tri.astype(ml_dtypes.bfloat16)}


def host_inputs_C(x1T_b, w_in, conv_w, hq):
    c0 = hq * 512
    wc = np.concatenate([w_in[:, c0:c0 + 512], w_in[:, 2048 + c0:2048 + c0 + 512], w_in[:, 4096 + c0:4096 + c0 + 512]], axis=1)
    wq = np.concatenate([w_in[:, 6144 + c0:6144 + c0 + 512], w_in[:, 8192 + c0:8192 + c0 + 512], w_in[:, 10240 + c0:10240 + c0 + 512]], axis=1)
    cw = conv_w[:, c0:c0 + 512].reshape(3, 4, 128).transpose(2, 0, 1).reshape(128, 12)
    d = {"xT": x1T_b, "wc": np.ascontiguousarray(wc), "wq": np.ascontiguousarray(wq), "convw": np.ascontiguousarray(cw)}
    d.update(host_consts_C())
    return d


NHG = 12
HP = 64
GC = NHG * HP
WS_COLS = 2 * GC + 256 + NHG


def build_A(SB, NBATCH=2, DBG=9):
    TP = 256
    NTOK = NBATCH * SB
    NTILE = NTOK // TP
    nc = bass.Bass("TRN2", target_bir_lowering=False)
    dr = lambda n, s, d=F32, k="ExternalInput": nc.dram_tensor(n, list(s), d, kind=k).ap()
    xT = dr("xT", [D, NTOK])
    xTp = dr("xTp", [D, SB])
    wssd = dr("wssd", [D, WS_COLS])
    wpool = dr("wpool", [D, 512])
    cwb_d = dr("cwb", [128, 8 * 5])
    dtp_d = dr("dtp", [128, 2 * NHG])
    dsk_d = dr("dskip", [128, GC])
    nw_d = dr("normw", [128, GC])
    pw_d = dr("poolw", [512, 512])
    psc_d = dr("poolsc", [128, 4])
    psel_d = dr("psel", [128, 5])
    icnt_d = dr("invcnt0", [128, TP])
    ident_d = dr("ident", [128, 128])
    ut_d = dr("ut", [128, 128])
    lt_d = dr("lt", [128, 128])
    yssd = dr("yssd", [NTOK, GC], BF16, "ExternalOutput")
    ypool = dr("ypool", [512, SB], BF16, "ExternalOutput")

    with ExitStack() as es:
        P = Prog(nc, es)
        wres = P.sbuf("wres", [128, KC, WS_COLS], BF16)
        xb_t = [es.enter_context(nc.sbuf_tensor(f"xb{i}", [128, KC, TP], BF16)) for i in range(2)]
        xb = [[Buf(f"xb{i}_{h}", xb_t[i]) for h in range(4)] for i in range(2)]
        xtok = [[P.token(f"xt{i}_{h}") for h in range(4)] for i in range(2)]
        es1 = ExitStack()
        sb = lambda n, s, d=F32: P.sbuf(n, s, d, es1)
        cwb = sb("cwb_s", [128, 8, 5]); dtp = sb("dtp_s", [128, 2 * NHG]); dsk = sb("dsk_s", [128, GC])
        nw = sb("nw_s", [128, GC]); ident = sb("ident_s", [128, 128]); ut = sb("ut_s", [128, 128]); lt = sb("lt_s", [128, 128])
        ones_f = sb("ones_f", [128, 128]); a_bc = sb("a_bc", [128, NHG])
        xpre = sb("xpre", [128, 8, TP + 3]); cacc = [sb(f"cacc{i}", [128, TP]) for i in range(2)]
        xc = sb("xc", [128, 8, TP]); bc16 = sb("bc16", [128, 2, TP], BF16)
        zs = sb("zs", [128, GC]); xtk = sb("xtk", [128, GC]); xdt = sb("xdt", [128, GC], BF16); xdt2 = sb("xdt2", [128, GC], BF16)
        btk = sb("btk", [128, 128], BF16); sml = sb("sml", [128, 8, NHG]); cbm = sb("cbm", [128, 128])
        lh = [sb(f"lh{i}", [128, 128]) for i in range(2)]; dec = [sb(f"dec{i}", [128, 128]) for i in range(2)]
        mh = [sb(f"mh{i}", [128, 128], BF16) for i in range(2)]
        St = sb("St", [128, GC]); S16 = sb("S16", [128, GC], BF16)
        yb = sb("yb", [128, GC]); ytmp = sb("ytmp", [128, GC]); yo = [sb(f"yo{i}", [128, GC], BF16) for i in range(2)]
        ss = sb("ss", [128, 4])
        ps = [P.psum(f"ps{i}", [128, 512]) for i in range(8)]
        ctok = P.token("const")
        wtok = [P.token(f"wtk{i}") for i in range(8)]
        otok = [P.token("o0"), P.token("o1")]
        for dst, src in ((dtp, dtp_d), (dsk, dsk_d), (nw, nw_d), (ident, ident_d), (ut, ut_d), (lt, lt_d)):
            P.dma("sp", ctok, dst[:, :], src[:, :], [], [dst])
        P.dma("sp", ctok, cwb[:, :, :], cwb_d.rearrange("p (c i) -> p c i", i=5), [], [cwb])
        P.op("dve", [], [ones_f], lambda e: e.memset(ones_f[:, :], 1.0))
        P.op("act", [dtp], [a_bc], lambda e: e.activation(out=a_bc[:, :], in_=dtp[:, NHG:2 * NHG], func=AF.Exp))
        P.op("dve", [a_bc], [a_bc], lambda e: e.tensor_scalar(out=a_bc[:, :], in0=a_bc[:, :], scalar1=-1.0, scalar2=None, op0=ALU.mult))
        xT_v = xT.rearrange("(c p) t -> p c t", p=128)
        xTp_v = xTp.rearrange("(c p) t -> p c t", p=128)
        ws_v = wssd.rearrange("(c p) f -> p c f", p=128)
        wp_v = wpool.rearrange("(c p) f -> p c f", p=128)
        xi = [0]

        def xload(src_v, tt):
            i = xi[0] % 2
            xi[0] += 1
            for h in range(4):
                P.dma("pool", xtok[i][h], xb_t[i][:, h * 8:(h + 1) * 8, :], src_v[:, h * 8:(h + 1) * 8, tt * TP:(tt + 1) * TP], [], [xb[i][h]])
            return xb_t[i], xb[i]

        bounds = [0, 256, 512, 768, 1024, 1280, 1536, 1792, WS_COLS]
        for q in range(8):
            P.dma("pool", wtok[q], wres[:, :, bounds[q]:bounds[q + 1]], ws_v[:, :, bounds[q]:bounds[q + 1]], [], [wres])

        bc3 = lambda ap: ap.unsqueeze(2).to_broadcast([128, ap.shape[1], HP])
        v3 = lambda ap: ap.rearrange("p (h d) -> p h d", d=HP)
        dt_t, da_t, ac_t, te_t, ea_t, et_t, dtt_t, tmp_t = (sml[:, i, :] for i in range(8))
        XO, BO, CO, DTO = GC, 2 * GC, 2 * GC + 128, 2 * GC + 256

        nxt = xload(xT_v, 0)
        for tt in range(NTILE):
            xt_, xbufs = nxt
            if tt + 1 < NTILE:
                nxt = xload(xT_v, tt + 1)
            if (tt * TP) % SB == 0:
                P.op("dve", [], [xpre], lambda e: e.memset(xpre[:, :, 0:3], 0.0))
                P.op("dve", [], [St], lambda e: e.memset(St[:, :], 0.0))
                P.op("dve", [], [S16], lambda e: e.memset(S16[:, :], 0.0))
            for ch in range(8):
                col = XO + ch * 128 if ch < 6 else (BO if ch == 6 else CO)
                pp = ps[ch % 2]
                for k in range(KC):
                    P.op("pe", xbufs + [wres], [pp], lambda e, pp=pp, col=col, k=k: _mm(
                        e, pp[:, :TP], wres[:, k, col:col + 128], xt_[:, k, :], k == 0, k == KC - 1))
                P.op("act", [pp], [xpre], lambda e, pp=pp, ch=ch: e.copy(out=xpre[:, ch, 3:TP + 3], in_=pp[:, :TP]))
                a_ = cacc[ch % 2]
                P.op("dve", [xpre, cwb], [a_], lambda e, a_=a_, ch=ch: e.tensor_scalar(out=a_[:, :], in0=xpre[:, ch, 0:TP], scalar1=cwb[:, ch, 0:1], scalar2=None, op0=ALU.mult))
                for i in range(1, 4):
                    P.op("dve", [xpre, cwb, a_], [a_], lambda e, a_=a_, ch=ch, i=i: e.scalar_tensor_tensor(
                        out=a_[:, :], in0=xpre[:, ch, i:TP + i], scalar=cwb[:, ch, i:i + 1], in1=a_[:, :], op0=ALU.mult, op1=ALU.add))
                P.op("act", [a_, cwb], [xc], lambda e, a_=a_, ch=ch: e.activation(out=xc[:, ch, :], in_=a_[:, :], func=AF.Silu, bias=cwb[:, ch, 4:5], scale=1.0))
                P.op("dve", [xpre], [xpre], lambda e, ch=ch: e.tensor_copy(out=xpre[:, ch, 0:3], in_=xpre[:, ch, TP:TP + 3]))
            P.op("pool", [xc], [bc16], lambda e: e.tensor_copy(out=bc16[:, :, :], in_=xc[:, 6:8, :]))
            for j in range(TP // 128 if DBG >= 2 else 0):
                csl = slice(j * 128, (j + 1) * 128)
                tok0 = tt * TP + j * 128
                for k in range(KC):
                    P.op("pe", xbufs + [wres], [ps[2]], lambda e, k=k: _mm(e, ps[2][:, 0:512], xt_[:, k, csl], wres[:, k, 0:512], k == 0, k == KC - 1))
                for k in range(KC):
                    P.op("pe", xbufs + [wres], [ps[3]], lambda e, k=k: _mm(e, ps[3][:, 0:256], xt_[:, k, csl], wres[:, k, 512:768], k == 0, k == KC - 1))
                if DBG < 2.02:
                    continue
                for k in range(KC):
                    P.op("pe", xbufs + [wres], [ps[3]], lambda e, k=k: _mm(e, ps[3][:, 256:256 + NHG], xt_[:, k, csl], wres[:, k, DTO:DTO + NHG], k == 0, k == KC - 1))
                if DBG < 2.04:
                    continue
                P.op("act", [ps[2]], [zs], lambda e: e.activation(out=zs[:, 0:512], in_=ps[2][:, 0:512], func=AF.Silu))
                P.op("act", [ps[3]], [zs], lambda e: e.activation(out=zs[:, 512:768], in_=ps[3][:, 0:256], func=AF.Silu))
                if DBG < 2.06:
                    continue
                P.op("dve", [ps[3], dtp], [sml], lambda e: e.tensor_tensor(out=dt_t, in0=ps[3][:, 256:256 + NHG], in1=dtp[:, 0:NHG], op=ALU.add))
                if DBG < 2.062:
                    continue
                P.op("act", [sml], [sml], lambda e: e.activation(out=dt_t, in_=dt_t, func=AF.Exp))
                if DBG < 2.063:
                    continue
                P.op("act", [sml], [sml], lambda e: e.activation(out=dt_t, in_=dt_t, func=AF.Ln, bias=1.0))
                if DBG < 2.064:
                    continue
                P.op("dve", [sml, a_bc], [sml], lambda e: e.tensor_tensor(out=da_t, in0=dt_t, in1=a_bc[:, :], op=ALU.mult))
                if DBG < 2.2:
                    continue
                for ch in range(6):
                    pb_, off = (ps[4], ch * 128) if ch < 4 else (ps[5], (ch - 4) * 128)
                    P.op("pe", [xc, ident], [pb_], lambda e, pb_=pb_, off=off, ch=ch: e.transpose(pb_[:, off:off + 128], xc[:, ch, csl], ident[:, :]))
                P.op("pe", [xc, ident], [ps[5]], lambda e: e.transpose(ps[5][:, 256:384], xc[:, 6, csl], ident[:, :]))
                P.op("act", [ps[4]], [xtk], lambda e: e.copy(out=xtk[:, 0:512], in_=ps[4][:, 0:512]))
                P.op("act", [ps[5]], [xtk], lambda e: e.copy(out=xtk[:, 512:768], in_=ps[5][:, 0:256]))
                P.op("dve", [ps[5]], [btk], lambda e: e.tensor_copy(out=btk[:, :], in_=ps[5][:, 256:384]))
                if DBG < 2.3:
                    continue
                P.op("pe", [lt, sml], [ps[3]], lambda e: _mm(e, ps[3][:, 272:272 + NHG], lt[:, :], da_t, True, True))
                P.op("pe", [ones_f, sml], [ps[3]], lambda e: _mm(e, ps[3][:, 288:288 + NHG], ones_f[:, :], da_t, True, True))
                P.op("pe", [bc16], [ps[3]], lambda e: _mm(e, ps[3][:, 384:512], bc16[:, 0, csl], bc16[:, 1, csl], True, True))
                P.op("dve", [ps[3]], [sml], lambda e: e.tensor_copy(out=ac_t, in_=ps[3][:, 272:272 + NHG]))
                P.op("dve", [ps[3], sml], [sml], lambda e: e.tensor_tensor(out=te_t, in0=ps[3][:, 288:288 + NHG], in1=ac_t, op=ALU.subtract))
                P.op("act", [sml], [sml], lambda e: e.activation(out=te_t, in_=te_t, func=AF.Exp))
                P.op("act", [sml], [sml], lambda e: e.activation(out=ea_t, in_=ac_t, func=AF.Exp))
                P.op("act", [ps[3]], [sml], lambda e: e.activation(out=et_t, in_=ps[3][:, 288:288 + NHG], func=AF.Exp))
                P.op("dve", [sml], [sml], lambda e: e.tensor_tensor(out=dtt_t, in0=dt_t, in1=te_t, op=ALU.mult))
                P.op("dve", [ps[3], lt], [cbm], lambda e: e.tensor_tensor(out=cbm[:, :], in0=ps[3][:, 384:512], in1=lt[:, :], op=ALU.mult))
                if DBG < 2.4:
                    continue
                P.op("dve", [xtk, sml], [xdt], lambda e: e.tensor_tensor(out=v3(xdt[:, :]), in0=v3(xtk[:, :]), in1=bc3(dt_t), op=ALU.mult))
                P.op("dve", [xtk, sml], [xdt2], lambda e: e.tensor_tensor(out=v3(xdt2[:, :]), in0=v3(xtk[:, :]), in1=bc3(dtt_t), op=ALU.mult))
                if DBG < 2.5:
                    continue
                P.op("pe", [bc16, S16], [ps[6]], lambda e: _mm(e, ps[6][:, 0:512], bc16[:, 1, csl], S16[:, 0:512], True, True))
                P.op("pe", [bc16, S16], [ps[7]], lambda e: _mm(e, ps[7][:, 0:256], bc16[:, 1, csl], S16[:, 512:768], True, True))
                P.op("pe", [btk, xdt2], [ps[2]], lambda e: _mm(e, ps[2][:, 0:512], btk[:, :], xdt2[:, 0:512], True, True))
                P.op("pe", [btk, xdt2], [ps[3]], lambda e: _mm(e, ps[3][:, 0:256], btk[:, :], xdt2[:, 512:768], True, True))
                if DBG < 2.6:
                    continue
                for h in range(NHG):
                    l_, d_, m_ = lh[h % 2], dec[h % 2], mh[h % 2]
                    pseg = ps[h % 2]
                    P.op("dve", [ut, sml], [l_], lambda e, l_=l_, h=h: e.tensor_scalar(out=l_[:, :], in0=ut[:, :], scalar1=da_t[:, h:h + 1], scalar2=None, op0=ALU.mult))
                    P.op("pe", [l_, lt], [pseg], lambda e, l_=l_, pseg=pseg: _mm(e, pseg[:, 0:128], l_[:, :], lt[:, :], True, True))
                    P.op("act", [pseg], [d_], lambda e, d_=d_, pseg=pseg: e.activation(out=d_[:, :], in_=pseg[:, 0:128], func=AF.Exp))
                    P.op("dve", [d_, cbm], [m_], lambda e, d_=d_, m_=m_: e.tensor_tensor(out=m_[:, :], in0=d_[:, :], in1=cbm[:, :], op=ALU.mult))
                    py, off = (ps[4], h * HP) if h < 8 else (ps[5], (h - 8) * HP)
                    P.op("pe", [m_, xdt], [py], lambda e, m_=m_, py=py, off=off, h=h: _mm(e, py[:, off:off + HP], m_[:, :], xdt[:, h * HP:(h + 1) * HP], True, True))
                if DBG < 2.7:
                    continue
                P.op("dve", [ps[6], sml], [yb], lambda e: e.tensor_tensor(out=v3(yb[:, 0:512]), in0=v3(ps[6][:, 0:512]), in1=bc3(ea_t[:, 0:8]), op=ALU.mult))
                P.op("dve", [ps[7], sml], [yb], lambda e: e.tensor_tensor(out=v3(yb[:, 512:768]), in0=v3(ps[7][:, 0:256]), in1=bc3(ea_t[:, 8:12]), op=ALU.mult))
                P.op("dve", [ps[4], yb], [yb], lambda e: e.tensor_tensor(out=yb[:, 0:512], in0=yb[:, 0:512], in1=ps[4][:, 0:512], op=ALU.add))
                P.op("dve", [ps[5], yb], [yb], lambda e: e.tensor_tensor(out=yb[:, 512:768], in0=yb[:, 512:768], in1=ps[5][:, 0:256], op=ALU.add))
                P.op("pool", [xtk, dsk], [ytmp], lambda e: e.tensor_tensor(out=ytmp[:, :], in0=xtk[:, :], in1=dsk[:, :], op=ALU.mult))
                P.op("dve", [yb, ytmp], [yb], lambda e: e.tensor_tensor(out=yb[:, :], in0=yb[:, :], in1=ytmp[:, :], op=ALU.add))
                P.op("dve", [yb, zs], [yb], lambda e: e.tensor_tensor(out=yb[:, :], in0=yb[:, :], in1=zs[:, :], op=ALU.mult))
                P.op("pool", [yb], [ytmp], lambda e: e.tensor_tensor(out=ytmp[:, :], in0=yb[:, :], in1=yb[:, :], op=ALU.mult))
                P.op("dve", [ytmp], [ss], lambda e: e.reduce_sum(out=ss[:, 0:1], in_=ytmp[:, :], axis=AX.X))
                P.op("dve", [ss], [ss], lambda e: e.tensor_scalar(out=ss[:, 1:2], in0=ss[:, 0:1], scalar1=1.0 / GC, scalar2=RMS_EPS, op0=ALU.mult, op1=ALU.add))
                P.op("act", [ss], [ss], lambda e: e.activation(out=ss[:, 2:3], in_=ss[:, 1:2], func=AF.Ln))
                P.op("act", [ss], [ss], lambda e: e.activation(out=ss[:, 3:4], in_=ss[:, 2:3], func=AF.Exp, scale=-0.5))
                y16 = yo[j % 2]
                P.op("dve", [yb, ss, nw], [y16], lambda e, y16=y16: e.scalar_tensor_tensor(out=y16[:, :], in0=yb[:, :], scalar=ss[:, 3:4], in1=nw[:, :], op0=ALU.mult, op1=ALU.mult))
                P.dma("sp", otok[j % 2], yssd[tok0:tok0 + 128, :], y16[:, :], [y16], [])
                if DBG < 2.8:
                    continue
                P.op("dve", [St, sml], [St], lambda e: e.tensor_tensor(out=v3(St[:, :]), in0=v3(St[:, :]), in1=bc3(et_t), op=ALU.mult))
                P.op("dve", [St, ps[2]], [St], lambda e: e.tensor_tensor(out=St[:, 0:512], in0=St[:, 0:512], in1=ps[2][:, 0:512], op=ALU.add))
                P.op("dve", [St, ps[3]], [St], lambda e: e.tensor_tensor(out=St[:, 512:768], in0=St[:, 512:768], in1=ps[3][:, 0:256], op=ALU.add))
                P.op("pool", [St], [S16], lambda e: e.tensor_copy(out=S16[:, :], in_=St[:, :]))

        P.barrier(wtok + otok + [t for tl_ in xtok for t in tl_])
        es1.close()
        sb = lambda n, s, d=F32: P.sbuf(n, s, d)
        wres_t = wres.t
        wp = Buf("wp", wres_t[:, :, 0:512])
        pw = P.sbuf("pw", [128, 4, 512], BF16)
        psc = sb("psc_s", [128, 4]); psel = sb("psel_s", [128, 5]); icnt = sb("icnt_s", [128, TP])
        W = TP + 16
        ub = sb("ub", [128, 4, W]); sA = sb("sA", [128, 4, W]); sB_ = sb("sB", [128, 4, W])
        pac = sb("pac", [128, 4, TP]); m16 = sb("m16", [128, 4, TP], BF16); yp = [sb(f"yp{i}", [128, 4, TP], BF16) for i in range(2)]
        for dst, src in ((psc, psc_d), (psel, psel_d), (icnt, icnt_d)):
            P.dma("sp", ctok, dst[:, :], src[:, :], [], [dst])
        P.dma("pool", wtok[0], pw[:, :, :], pw_d.rearrange("(c p) f -> p c f", p=128), [], [pw])
        for q in range(2):
            P.dma("pool", wtok[1 + q], wp[:, :, q * 256:(q + 1) * 256], wp_v[:, :, q * 256:(q + 1) * 256], [], [wp])
        P.op("dve", [], [ub], lambda e: e.memset(ub[:, :, 0:16], 0.0))
        NPT = SB // TP if DBG >= 4 else 0
        nxt = xload(xTp_v, 0)
        for tt in range(NPT):
            xt_, xbufs = nxt
            if tt + 1 < NPT:
                nxt = xload(xTp_v, tt + 1)
            for c in range(4):
                pp = ps[c % 2]
                for k in range(KC):
                    P.op("pe", xbufs + [wp], [pp], lambda e, pp=pp, c=c, k=k: _mm(e, pp[:, :TP], wp[:, k, c * 128:(c + 1) * 128], xt_[:, k, :], k == 0, k == KC - 1))
                P.op("act", [pp], [ub], lambda e, pp=pp, c=c: e.copy(out=ub[:, c, 16:W], in_=pp[:, :TP]))
            P.op("dve", [ub], [sA], lambda e: e.tensor_tensor(out=sA[:, :, 1:W], in0=ub[:, :, 1:W], in1=ub[:, :, 0:W - 1], op=ALU.add))
            P.op("dve", [sA, psel], [pac], lambda e: e.tensor_scalar(out=pac[:, :, :], in0=sA[:, :, 16:W], scalar1=psel[:, 0:1], scalar2=None, op0=ALU.mult))
            P.op("dve", [sA], [sB_], lambda e: e.tensor_tensor(out=sB_[:, :, 3:W], in0=sA[:, :, 3:W], in1=sA[:, :, 1:W - 2], op=ALU.add))
            P.op("dve", [sB_, psel, pac], [pac], lambda e: e.scalar_tensor_tensor(out=pac[:, :, :], in0=sB_[:, :, 16:W], scalar=psel[:, 1:2], in1=pac[:, :, :], op0=ALU.mult, op1=ALU.add))
            P.op("dve", [sB_], [sA], lambda e: e.tensor_tensor(out=sA[:, :, 7:W], in0=sB_[:, :, 7:W], in1=sB_[:, :, 3:W - 4], op=ALU.add))
            P.op("dve", [sA, psel, pac], [pac], lambda e: e.scalar_tensor_tensor(out=pac[:, :, :], in0=sA[:, :, 16:W], scalar=psel[:, 2:3], in1=pac[:, :, :], op0=ALU.mult, op1=ALU.add))
            P.op("dve", [sA], [sB_], lambda e: e.tensor_tensor(out=sB_[:, :, 15:W], in0=sA[:, :, 15:W], in1=sA[:, :, 7:W - 8], op=ALU.add))
            P.op("dve", [sB_, psel, pac], [pac], lambda e: e.scalar_tensor_tensor(out=pac[:, :, :], in0=sB_[:, :, 16:W], scalar=psel[:, 3:4], in1=pac[:, :, :], op0=ALU.mult, op1=ALU.add))
            if tt == 0:
                P.op("dve", [pac, icnt], [pac], lambda e: e.tensor_tensor(out=pac[:, :, :], in0=pac[:, :, :], in1=icnt[:, :].unsqueeze(1).to_broadcast([128, 4, TP]), op=ALU.mult))
                P.op("dve", [pac, ub], [m16], lambda e: e.tensor_tensor(out=m16[:, :, :], in0=pac[:, :, :], in1=ub[:, :, 16:W], op=ALU.subtract))
            else:
                P.op("dve", [pac, ub, psel], [m16], lambda e: e.scalar_tensor_tensor(out=m16[:, :, :], in0=pac[:, :, :], scalar=psel[:, 4:5], in1=ub[:, :, 16:W], op0=ALU.mult, op1=ALU.subtract))
            P.op("dve", [ub], [ub], lambda e: e.tensor_copy(out=ub[:, :, 0:16], in_=ub[:, :, TP:W]))
            y = yp[tt % 2]
            for dch in range(4):
                pp = ps[2 + dch % 2]
                for k in range(4):
                    P.op("pe", [pw, m16], [pp], lambda e, pp=pp, dch=dch, k=k: _mm(e, pp[:, :TP], pw[:, k, dch * 128:(dch + 1) * 128], m16[:, k, :], k == 0, k == 3))
                P.op("act", [pp, psc], [y], lambda e, pp=pp, dch=dch, y=y: e.activation(out=y[:, dch, :], in_=pp[:, :TP], func=AF.Copy, scale=psc[:, dch:dch + 1]))
            P.dma("sp", otok[tt % 2], ypool[:, tt * TP:(tt + 1) * TP].rearrange("(c p) t -> p c t", p=128), y[:, :, :], [y], [])
        P.finish("sp", otok)
    return nc


POOL_WINDOWS = (2, 4, 8, 16)


def host_inputs_A(xT_full, xT_b, w_in, conv_w, conv_b, dt_bias, a_log, d_skip, norm_w, pool_w, pool_scale, g, pg, TP=256):
    zc = 2048 + g * GC
    xcol = 8192 + g * GC
    bcol = 8192 + 6144 + g * 128
    ccol = 8192 + 6144 + 1024 + g * 128
    dcol = 16384 + g * NHG
    wssd = np.concatenate([w_in[:, zc:zc + GC], w_in[:, xcol:xcol + GC], w_in[:, bcol:bcol + 128], w_in[:, ccol:ccol + 128], w_in[:, dcol:dcol + NHG]], axis=1)
    wpool = w_in[:, pg * 512:(pg + 1) * 512]
    cidx = np.concatenate([np.arange(g * GC, (g + 1) * GC), 6144 + g * 128 + np.arange(128), 6144 + 1024 + g * 128 + np.arange(128)])
    cw = conv_w[:, cidx]
    cb = conv_b[cidx]
    cwb = np.concatenate([cw, cb[None, :]], axis=0)
    cwb = cwb.reshape(5, 8, 128).transpose(2, 1, 0).reshape(128, 40)
    hs = slice(g * NHG, (g + 1) * NHG)
    dtp = np.broadcast_to(np.concatenate([dt_bias[hs], a_log[hs]])[None, :], (128, 2 * NHG))
    dsk = np.broadcast_to(np.repeat(d_skip[hs], HP)[None, :], (128, GC))
    nwb = np.broadcast_to(norm_w[g * GC:(g + 1) * GC][None, :], (128, GC))
    psc = pool_scale[pg * 512:(pg + 1) * 512].reshape(4, 128).T
    w = POOL_WINDOWS[pg]
    psel = np.zeros((128, 5), np.float32)
    psel[:, pg] = 1.0
    psel[:, 4] = 1.0 / w
    pos = np.arange(1, TP + 1, dtype=np.float32)
    icnt = np.broadcast_to((1.0 / np.minimum(pos, float(w)))[None, :], (128, TP))
    k = np.arange(128)
    ca = lambda a: np.ascontiguousarray(a, dtype=np.float32)
    return {
        "xT": xT_full, "xTp": xT_b, "wssd": ca(wssd), "wpool": ca(wpool), "cwb": ca(cwb), "dtp": ca(dtp),
        "dskip": ca(dsk), "normw": ca(nwb), "poolw": ca(pool_w[pg]), "poolsc": ca(psc), "psel": psel, "invcnt0": ca(icnt),
        "ident": np.eye(128, dtype=np.float32), "ut": ca(k[:, None] > k[None, :]), "lt": ca(k[:, None] <= k[None, :]),
    }


NCORES = 8
BATCH = 2
SEQ = 8192
_PROG_CACHE = {}


def _prog(key, fn):
    if key not in _PROG_CACHE:
        _PROG_CACHE[key] = fn()
    return _PROG_CACHE[key]


def _run(nc, in_maps):
    res = run_bass_kernel_spmd(nc, in_maps, core_ids=list(range(len(in_maps))))
    return res.results


def _layer_F(ymixT, xresT, KM, w_out, ln_mix_g, ln_mix_b, ln_ffn_g, ln_ffn_b, router_w, router_b, w1, b1, w2, b2):
    NTOK = xresT.shape[1]
    NT = NTOK // NCORES
    nc = _prog(("F", NT, KM), lambda: build_F(NT, KM))
    maps = []
    for c in range(NCORES):
        sl = slice(c * NT, (c + 1) * NT)
        maps.append(host_inputs_F(np.ascontiguousarray(ymixT[:, sl]), np.ascontiguousarray(xresT[:, sl]), w_out,
                                  ln_mix_g, ln_mix_b, ln_ffn_g, ln_ffn_b, router_w, router_b, w1, b1, w2, b2))
    outs = _run(nc, maps)
    return np.concatenate([o["out"] for o in outs], axis=1)


def kernel(x,
           l0_w_in, l0_conv_w, l0_conv_b, l0_dt_bias, l0_a_log, l0_d_skip, l0_ssm_norm_w,
           l0_pool_w, l0_pool_scale, l0_w_out, l0_ln_mix_g, l0_ln_mix_b,
           l0_router_w, l0_router_b, l0_w1, l0_b1, l0_w2, l0_b2, l0_ln_ffn_g, l0_ln_ffn_b,
           l1_w_in, l1_conv_w, l1_w_out, l1_ln_mix_g, l1_ln_mix_b,
           l1_router_w, l1_router_b, l1_w1, l1_b1, l1_w2, l1_b2, l1_ln_ffn_g, l1_ln_ffn_b):
    import ml_dtypes
    f32 = lambda a: np.asarray(a, dtype=np.float32)
    x = f32(x)
    B, S, _ = x.shape
    NTOK = B * S
    xT = np.ascontiguousarray(x.reshape(NTOK, D).T)

    ncA = _prog(("A", S), lambda: build_A(S, B))
    w_in0 = f32(l0_w_in)
    maps = []
    for c in range(NCORES):
        b, pg = c // 4, c % 4
        maps.append(host_inputs_A(xT, np.ascontiguousarray(xT[:, b * S:(b + 1) * S]), w_in0, f32(l0_conv_w), f32(l0_conv_b),
                                  f32(l0_dt_bias), f32(l0_a_log), f32(l0_d_skip), f32(l0_ssm_norm_w), f32(l0_pool_w),
                                  f32(l0_pool_scale), c, pg))
    outs = _run(ncA, maps)
    ymix0 = np.empty((8192, NTOK), dtype=ml_dtypes.bfloat16)
    for c in range(NCORES):
        b, pg = c // 4, c % 4
        ymix0[pg * 512:(pg + 1) * 512, b * S:(b + 1) * S] = outs[c]["ypool"]
        ymix0[2048 + c * GC:2048 + (c + 1) * GC, :] = outs[c]["yssd"].T
    del outs, maps

    x1T = _layer_F(ymix0, xT, 8192, f32(l0_w_out), f32(l0_ln_mix_g), f32(l0_ln_mix_b), f32(l0_ln_ffn_g), f32(l0_ln_ffn_b),
                   f32(l0_router_w), f32(l0_router_b), f32(l0_w1), f32(l0_b1), f32(l0_w2), f32(l0_b2))
    del ymix0

    ncC = _prog(("C", S), lambda: build_C(S))
    w_in1 = f32(l1_w_in)
    maps = []
    for c in range(NCORES):
        b, hq = c // 4, c % 4
        maps.append(host_inputs_C(np.ascontiguousarray(x1T[:, b * S:(b + 1) * S]), w_in1, f32(l1_conv_w), hq))
    outs = _run(ncC, maps)
    ymix1 = np.empty((4096, NTOK), dtype=ml_dtypes.bfloat16)
    for c in range(NCORES):
        b, hq = c // 4, c % 4
        ymix1[hq * 512:(hq + 1) * 512, b * S:(b + 1) * S] = outs[c]["ymix"][0:512]
        ymix1[2048 + hq * 512:2048 + (hq + 1) * 512, b * S:(b + 1) * S] = outs[c]["ymix"][512:1024]
    del outs, maps

    x2T = _layer_F(ymix1, x1T, 4096, f32(l1_w_out), f32(l1_ln_mix_g), f32(l1_ln_mix_b), f32(l1_ln_ffn_g), f32(l1_ln_ffn_b),
                   f32(l1_router_w), f32(l1_router_b), f32(l1_w1), f32(l1_b1), f32(l1_w2), f32(l1_b2))
    return np.ascontiguousarray(x2T.T).reshape(B, S, D).astype(np.float32)
```

```python
import numpy as np
from contextlib import ExitStack
import concourse.bass as bass
import concourse.mybir as mybir
from concourse.bass_utils import run_bass_kernel_spmd

F32 = mybir.dt.float32
BF16 = mybir.dt.bfloat16
AF = mybir.ActivationFunctionType
ALU = mybir.AluOpType
AX = mybir.AxisListType

D = 4096
KC = D // 128
NE = 32
TOPK = 4
DEXP = 512
ALPHA = 4.0 ** 0.25
LN_EPS = 1e-5
RMS_EPS = 1e-5
LIMIT = 7.0
SW_ALPHA = 1.702


class Chan:
    def __init__(self, prog, name, step):
        self.prog, self.name, self.step = prog, name, step
        self.ep = 60000 if step == 1 else 3700
        self.sems = []
        self.cnt = 0

    def sem_val(self, n):
        k = (n - 1) // self.ep
        while len(self.sems) <= k:
            self.sems.append(self.prog.new_sem(f"{self.name}_{len(self.sems)}"))
        return self.sems[k], ((n - 1) % self.ep + 1) * self.step


class Buf:
    def __init__(self, name, t=None, excl=False):
        self.name, self.t = name, t
        self.w = None
        self.r = {}
        self.excl = excl

    def __getitem__(self, k):
        return self.t[k]


class Prog:
    def __init__(self, nc, es, same_engine_sync=True):
        self.nc, self.es = nc, es
        self.E = {"pe": nc.tensor, "act": nc.scalar, "dve": nc.vector, "pool": nc.gpsimd, "sp": nc.sync}
        self.nsem = 0
        self.chan = {k: Chan(self, k, 1) for k in ("pe", "act", "dve", "pool")}
        self.seen = {k: {} for k in self.E}
        self.ses = same_engine_sync
        self.ntok = 0
        self.toks = {}
        self.cur_es = es
        self.nbuf = 0

    def new_sem(self, name):
        self.nsem += 1
        return self.es.enter_context(self.nc.semaphore(f"s_{name}_{self.nsem}"))

    def sbuf(self, name, shape, dt, es=None):
        self.nbuf += 1
        return Buf(name, (es or self.cur_es).enter_context(self.nc.sbuf_tensor(f"{name}_{self.nbuf}", list(shape), dt)))

    def make_psum(self):
        return [self.psum(f"ps{i}", [128, 512]) for i in range(8)]

    def psum(self, name, shape, dt=F32):
        return Buf(name, self.es.enter_context(self.nc.psum_tensor(name, list(shape), dt)), excl=True)

    def token(self, name=None):
        self.ntok += 1
        name = name or f"tok{self.ntok}"
        if name not in self.toks:
            self.toks[name] = Chan(self, name, 16)
        return self.toks[name]

    def _wait(self, eng, deps):
        for ch, n in deps:
            if ch is self.chan.get(eng) and (eng == "pe" or not self.ses):
                continue
            if self.seen[eng].get(ch, 0) >= n:
                continue
            s, v = ch.sem_val(n)
            self.E[eng].wait_ge(s, v)
            self.seen[eng][ch] = n

    @staticmethod
    def _deps(reads, writes, own=None):
        deps = set()
        for b in reads:
            if b.w:
                deps.add(b.w)
            if b.excl:
                for ch, ev in b.r.items():
                    if ch is not own:
                        deps.add(ev)
        for b in writes:
            if b.w:
                deps.add(b.w)
            for ev in b.r.values():
                deps.add(ev)
        return deps

    def op(self, eng, reads, writes, fn):
        own = self.chan[eng]
        deps = self._deps(reads, writes, own)
        self._wait(eng, deps)
        inst = fn(self.E[eng])
        own.cnt += 1
        s, v = own.sem_val(own.cnt)
        inst.then_inc(s, 1)
        ev = (own, own.cnt)
        for b in reads:
            b.r[own] = ev
        for b in writes:
            b.w = ev
            b.r = {}
        return ev

    def dma(self, q, tok, out, in_, reads=(), writes=()):
        deps = self._deps(reads, writes)
        if tok.cnt:
            deps.add((tok, tok.cnt))
        self._wait(q, deps)
        inst = self.E[q].dma_start(out=out, in_=in_)
        tok.cnt += 1
        s, v = tok.sem_val(tok.cnt)
        inst.then_inc(s, 16)
        ev = (tok, tok.cnt)
        for b in reads:
            b.r[tok] = ev
        for b in writes:
            b.w = ev
            b.r = {}
        return ev

    def barrier(self, toks=()):
        evs = {(ch, ch.cnt) for ch in self.chan.values() if ch.cnt}
        evs |= {(t, t.cnt) for t in self.toks.values() if t.cnt}
        for eng in self.E:
            self._wait(eng, evs)

    def finish(self, eng, toks=None):
        self._wait(eng, {(t, t.cnt) for t in self.toks.values() if t.cnt})


def _mm(pe, out, lhsT, rhs, start, stop):
    return pe.matmul(out, lhsT, rhs, start=start, stop=stop)


def emit_layernorm(P, C, r, TT, g_ap, b_ap, ps_a, ps_b, sq, stat, x16=None):
    ones = C["ones_f"]
    for c in range(KC):
        P.op("pe", [r, ones], [ps_a], lambda e, c=c: _mm(e, ps_a[:, :TT], ones[:, :], r[:, c, :], c == 0, c == KC - 1))
    for c in range(KC):
        s = sq[c % 2]
        P.op("act", [r], [s], lambda e, c=c, s=s: e.activation(out=s[:, :], in_=r[:, c, :], func=AF.Square))
        P.op("pe", [s, ones], [ps_b], lambda e, c=c, s=s: _mm(e, ps_b[:, :TT], ones[:, :], s[:, :], c == 0, c == KC - 1))
    mean, var, rstd, nmr = (stat[:, i, :] for i in range(4))
    P.op("dve", [ps_a], [stat], lambda e: e.tensor_scalar(out=mean, in0=ps_a[:, :TT], scalar1=1.0 / D, scalar2=None, op0=ALU.mult))
    P.op("dve", [stat], [stat], lambda e: e.tensor_tensor(out=nmr, in0=mean, in1=mean, op=ALU.mult))
    P.op("dve", [ps_b, stat], [stat], lambda e: e.scalar_tensor_tensor(out=var, in0=ps_b[:, :TT], scalar=1.0 / D, in1=nmr, op0=ALU.mult, op1=ALU.subtract))
    P.op("dve", [stat], [stat], lambda e: e.tensor_scalar(out=var, in0=var, scalar1=LN_EPS, scalar2=None, op0=ALU.add))
    P.op("act", [stat], [stat], lambda e: e.activation(out=var, in_=var, func=AF.Ln))
    P.op("act", [stat], [stat], lambda e: e.activation(out=rstd, in_=var, func=AF.Exp, scale=-0.5))
    P.op("dve", [stat], [stat], lambda e: e.scalar_tensor_tensor(out=nmr, in0=mean, scalar=-1.0, in1=rstd, op0=ALU.mult, op1=ALU.mult))
    for c in range(KC):
        s = sq[c % 2]
        P.op("dve", [r, stat], [s], lambda e, c=c, s=s: e.tensor_tensor(out=s[:, :], in0=r[:, c, :], in1=rstd, op=ALU.mult))
        P.op("dve", [s, stat], [s], lambda e, s=s: e.tensor_tensor(out=s[:, :], in0=s[:, :], in1=nmr, op=ALU.add))
        P.op("act", [s, C["lnp"]], [r], lambda e, c=c, s=s: e.activation(out=r[:, c, :], in_=s[:, :], func=AF.Identity, scale=g_ap(c), bias=b_ap(c)))
        if x16 is not None:
            P.op("pool", [r], [x16], lambda e, c=c: e.tensor_copy(out=x16[:, c, :], in_=r[:, c, :]))


def emit_F(nc, P, ps, NT, KM, io, TT=512):
    KMC = KM // 128
    NTT = NT // TT
    NSUB = TT // 128
    NH1 = max(1, KMC // 32)
    ymix, xres, w_out, lnp_d, rw_d, rb_d = (io[k] for k in ("ymix", "xres", "w_out", "lnp", "rw", "rb"))
    w1, b1_d, w2, b2_d, ident_d, out = (io[k] for k in ("w1", "b1t", "w2", "b2", "ident", "out"))

    with ExitStack() as es:
        P.cur_es = es
        r = P.sbuf("r", [128, KC, TT], F32)
        big = P.sbuf("big", [128, 32, TT], BF16)
        x16 = P.sbuf("x16", [128, KC, TT], BF16)
        NW = 3
        wbt = [P.sbuf(f"wb{i}", [128, 8, 512], BF16).t for i in range(NW)]
        wbh = [[Buf(f"wb{i}a", wbt[i]), Buf(f"wb{i}b", wbt[i])] for i in range(NW)]
        wtok = [[P.token(f"wt{i}a"), P.token(f"wt{i}b")] for i in range(NW)]
        sq = [P.sbuf(f"sq{i}", [128, TT], F32) for i in range(2)]
        stat = P.sbuf("stat", [128, 4, TT], F32)
        tg = [P.sbuf(f"tg{i}", [128, TT], F32) for i in range(2)]
        tl = [P.sbuf(f"tl{i}", [128, TT], F32) for i in range(2)]
        tsg = [P.sbuf(f"tsg{i}", [128, TT], F32) for i in range(2)]
        lnp = P.sbuf("lnp_s", [128, 4 * KC], F32)
        rw = P.sbuf("rw_s", [128, KC * NE], F32)
        rb = P.sbuf("rb_s", [128, NE], F32)
        b1t = P.sbuf("b1t_s", [128, NE * 8], F32)
        b2p = [P.sbuf(f"b2p{i}", [NE, 512], F32) for i in range(2)]
        b2tok = [P.token(f"b2t{i}") for i in range(2)]
        ident = P.sbuf("ident_s", [128, 128], F32)
        ones_f = P.sbuf("ones_f", [128, 128], F32)
        lg = P.sbuf("lg", [128, NSUB, NE], F32)
        gate = P.sbuf("gate", [128, NSUB, NE], F32)
        sm = P.sbuf("sm", [128, NSUB, 16], F32)
        gT = P.sbuf("gT", [NE, TT], F32)
        gTm = [P.sbuf(f"gTm{i}", [NE, TT], F32) for i in range(2)]
        C = {"ones_f": ones_f, "lnp": lnp}
        pset = [[ps[0], ps[1], ps[2], ps[3]], [ps[4], ps[5], ps[7], ps[6]]]
        gbc = [P.sbuf(f"gbc{i}", [128, TT], F32) for i in range(2)]
        ctok = P.token("const")
        iotok = [P.token("io0"), P.token("io1"), P.token("io2")]
        otok = P.token("otok")

        for dst, src in ((lnp, lnp_d), (rw, rw_d), (rb, rb_d), (b1t, b1_d), (ident, ident_d)):
            P.dma("sp", ctok, dst[:, :], src[:, :], [], [dst])
        P.op("dve", [], [ones_f], lambda e: e.memset(ones_f[:, :], 1.0))

        ymix_v = ymix.rearrange("(c p) t -> p c t", p=128)
        xres_v = xres.rearrange("(c p) t -> p c t", p=128)
        out_v = out.rearrange("(c p) t -> p c t", p=128)
        wout_v = w_out.rearrange("(c p) d -> p c d", p=128)
        w1_v = w1.rearrange("e (c p) f -> p e c f", p=128)
        w2_v = w2.rearrange("e (c p) d -> p e c d", p=128)

        wi = [0]

        def wload(halves):
            i = wi[0] % NW
            wi[0] += 1
            for h, (dsl, src) in enumerate(halves):
                P.dma("pool", wtok[i][h], dsl(wbt[i]), src, [], [wbh[i][h]])
            return wbt[i], wbh[i]

        def run_stream(items, depth=2):
            loaded = []
            n = len(items)
            for i in range(min(depth, n)):
                loaded.append(wload(items[i][0]))
            for i in range(n):
                wt, wbufs = loaded[i]
                items[i][1](wt, wbufs)
                if i + depth < n:
                    loaded.append(wload(items[i + depth][0]))

        for tt in range(NTT):
            t0 = tt * TT
            tsl = slice(t0, t0 + TT)
            P.dma("sp", iotok[2], r[:, :, :], xres_v[:, :, tsl], [], [r])
            for hf in range(NH1):
                kc0 = hf * 32
                nk = min(32, KMC)
                for h in range(2):
                    cs = slice(h * nk // 2, (h + 1) * nk // 2)
                    P.dma("sp", iotok[h], big[:, cs, :], ymix_v[:, kc0 + cs.start:kc0 + cs.stop, tsl], [], [big])
                items = []
                for g in range(8):
                    dsl = slice(g * 512, (g + 1) * 512)
                    for kg in range(nk // 8):
                        k0 = kc0 + kg * 8
                        halves = [(lambda b: b[:, 0:4, :], wout_v[:, k0:k0 + 4, dsl]),
                                  (lambda b: b[:, 4:8, :], wout_v[:, k0 + 4:k0 + 8, dsl])]

                        def comp(wt, wbufs, g=g, kg=kg, hf=hf):
                            pb = pset[g % 2]
                            for kc in range(8):
                                k = kg * 8 + kc
                                for j in range(4):
                                    P.op("pe", wbufs + [big], [pb[j]], lambda e, kc=kc, j=j, k=k: _mm(
                                        e, pb[j][:, :TT], wt[:, kc, j * 128:(j + 1) * 128], big[:, k, :], k == 0, k == nk - 1))
                            if kg == nk // 8 - 1:
                                for j in range(4):
                                    c = g * 4 + j
                                    if hf == 0:
                                        P.op("dve", [r, pb[j]], [r], lambda e, c=c, j=j: e.scalar_tensor_tensor(
                                            out=r[:, c, :], in0=r[:, c, :], scalar=ALPHA, in1=pb[j][:, :TT], op0=ALU.mult, op1=ALU.add))
                                    else:
                                        P.op("dve", [r, pb[j]], [r], lambda e, c=c, j=j: e.tensor_tensor(
                                            out=r[:, c, :], in0=r[:, c, :], in1=pb[j][:, :TT], op=ALU.add))
                        items.append((halves, comp))
                run_stream(items)
            emit_layernorm(P, C, r, TT, lambda c: lnp[:, c:c + 1], lambda c: lnp[:, KC + c:KC + c + 1],
                           ps[4], ps[5], sq, stat, x16)
            for s in range(NSUB):
                for c in range(KC):
                    P.op("pe", [r, rw], [ps[6]], lambda e, s=s, c=c: _mm(
                        e, ps[6][:, s * NE:(s + 1) * NE], r[:, c, s * 128:(s + 1) * 128], rw[:, c * NE:(c + 1) * NE], c == 0, c == KC - 1))
            for s in range(NSUB):
                P.op("dve", [ps[6], rb], [lg], lambda e, s=s: e.tensor_tensor(out=lg[:, s, :], in0=ps[6][:, s * NE:(s + 1) * NE], in1=rb[:, :], op=ALU.add))
                P.op("dve", [lg], [sm], lambda e, s=s: e.max(out=sm[:, s, 0:8], in_=lg[:, s, :]))
                P.op("dve", [sm], [sm], lambda e, s=s: e.tensor_scalar(out=sm[:, s, 8:9], in0=sm[:, s, 0:1], scalar1=-1.0, scalar2=None, op0=ALU.mult))
                P.op("dve", [lg, sm], [gate], lambda e, s=s: e.tensor_scalar(out=gate[:, s, :], in0=lg[:, s, :], scalar1=sm[:, s, 3:4], scalar2=None, op0=ALU.is_ge))
                P.op("act", [lg, sm], [lg], lambda e, s=s: e.activation(out=lg[:, s, :], in_=lg[:, s, :], func=AF.Exp, bias=sm[:, s, 8:9], scale=1.0))
                P.op("dve", [lg, gate], [gate], lambda e, s=s: e.tensor_tensor(out=gate[:, s, :], in0=gate[:, s, :], in1=lg[:, s, :], op=ALU.mult))
                P.op("dve", [gate], [sm], lambda e, s=s: e.reduce_sum(out=sm[:, s, 9:10], in_=gate[:, s, :], axis=AX.X))
                P.op("dve", [sm], [sm], lambda e, s=s: e.reciprocal(out=sm[:, s, 10:11], in_=sm[:, s, 9:10]))
                P.op("dve", [gate, sm], [gate], lambda e, s=s: e.tensor_scalar(out=gate[:, s, :], in0=gate[:, s, :], scalar1=sm[:, s, 10:11], scalar2=None, op0=ALU.mult))
                P.op("pe", [gate, ident], [ps[7]], lambda e, s=s: e.transpose(ps[7][0:NE, s * 128:(s + 1) * 128], gate[:, s, :], ident[:, :]))
            P.op("act", [ps[7]], [gT], lambda e: e.copy(out=gT[:, :], in_=ps[7][0:NE, :TT]))
            for ph in range(4):
                items = []
                for el in range(8):
                    ex = ph * 8 + el
                    for jp in range(2):
                        for kg in range(4):
                            ksl = slice(kg * 8, (kg + 1) * 8)
                            halves = [(lambda b: b[:, :, 0:256], w1_v[:, ex, ksl, jp * 256:(jp + 1) * 256]),
                                      (lambda b: b[:, :, 256:512], w1_v[:, ex, ksl, 512 + jp * 256:512 + (jp + 1) * 256])]

                            def comp(wt, wbufs, ex=ex, el=el, jp=jp, kg=kg):
                                pb = pset[(ex * 2 + jp) % 2]
                                gb = gbc[ex % 2]
                                if jp == 0 and kg == 0:
                                    gm = gTm[ex % 2]
                                    P.op("dve", [gT, ident], [gm], lambda e: e.tensor_scalar(
                                        out=gm[:, :], in0=gT[:, :], scalar1=ident[0:NE, ex:ex + 1], scalar2=None, op0=ALU.mult))
                                    P.op("pe", [ones_f, gm], [ps[6]], lambda e: _mm(e, ps[6][:, :TT], ones_f[0:NE, :], gm[:, :], True, True))
                                    P.op("act", [ps[6]], [gb], lambda e: e.copy(out=gb[:, :], in_=ps[6][:, :TT]))
                                for kc in range(8):
                                    k = kg * 8 + kc
                                    for q in range(4):
                                        P.op("pe", wbufs + [x16], [pb[q]], lambda e, kc=kc, q=q, k=k: _mm(
                                            e, pb[q][:, :TT], wt[:, kc, q * 128:(q + 1) * 128], x16[:, k, :], k == 0, k == KC - 1))
                                if kg == 3:
                                    for jj in range(2):
                                        j = jp * 2 + jj
                                        pg, pl = pb[jj], pb[2 + jj]
                                        a, l_ = tg[jj], tl[jj]
                                        bg = b1t[:, ex * 8 + j:ex * 8 + j + 1]
                                        bl = b1t[:, ex * 8 + 4 + j:ex * 8 + 4 + j + 1]
                                        P.op("dve", [pg, b1t], [a], lambda e, a=a, pg=pg, bg=bg: e.tensor_scalar(out=a[:, :], in0=pg[:, :TT], scalar1=bg, scalar2=LIMIT, op0=ALU.add, op1=ALU.min))
                                        P.op("dve", [pl, b1t], [l_], lambda e, l_=l_, pl=pl, bl=bl: e.tensor_scalar(out=l_[:, :], in0=pl[:, :TT], scalar1=bl, scalar2=LIMIT, op0=ALU.add, op1=ALU.min))
                                    for jj in range(2):
                                        j = jp * 2 + jj
                                        a, l_, sg = tg[jj], tl[jj], tsg[jj]
                                        P.op("act", [a], [sg], lambda e, a=a, sg=sg: e.activation(out=sg[:, :], in_=a[:, :], func=AF.Sigmoid, scale=SW_ALPHA))
                                        P.op("dve", [l_], [l_], lambda e, l_=l_: e.tensor_scalar(out=l_[:, :], in0=l_[:, :], scalar1=-LIMIT, scalar2=1.0, op0=ALU.max, op1=ALU.add))
                                        P.op("dve", [a, sg], [a], lambda e, a=a, sg=sg: e.tensor_tensor(out=a[:, :], in0=a[:, :], in1=sg[:, :], op=ALU.mult))
                                        P.op("dve", [a, l_], [a], lambda e, a=a, l_=l_: e.tensor_tensor(out=a[:, :], in0=a[:, :], in1=l_[:, :], op=ALU.mult))
                                        P.op("dve", [a, gb], [big], lambda e, a=a, j=j: e.tensor_tensor(out=big[:, el * 4 + j, :], in0=a[:, :], in1=gb[:, :], op=ALU.mult))
                            items.append((halves, comp))
                for g in range(8):
                    dsl = slice(g * 512, (g + 1) * 512)
                    for pc in range(4):
                        e0 = ph * 8 + pc * 2
                        halves = [(lambda b: b[:, 0:4, :], w2_v[:, e0, :, dsl]), (lambda b: b[:, 4:8, :], w2_v[:, e0 + 1, :, dsl])]

                        def comp(wt, wbufs, g=g, pc=pc, ph=ph, dsl=dsl):
                            pb = pset[g % 2]
                            if ph == 0 and pc == 0:
                                bp = b2p[g % 2]
                                P.dma("sp", b2tok[g % 2], bp[:, :], b2_d[:, dsl], [], [bp])
                                for j in range(4):
                                    P.op("pe", [bp, gT], [pb[j]], lambda e, j=j: _mm(
                                        e, pb[j][:, :TT], bp[:, j * 128:(j + 1) * 128], gT[:, :], True, False))
                            for kc in range(8):
                                k = pc * 8 + kc
                                for j in range(4):
                                    P.op("pe", wbufs + [big], [pb[j]], lambda e, kc=kc, j=j, k=k: _mm(
                                        e, pb[j][:, :TT], wt[:, kc, j * 128:(j + 1) * 128], big[:, k, :], (k == 0 and ph > 0), k == 31))
                            if pc == 3:
                                for j in range(4):
                                    c = g * 4 + j
                                    if ph == 0:
                                        P.op("dve", [r, pb[j]], [r], lambda e, c=c, j=j: e.scalar_tensor_tensor(
                                            out=r[:, c, :], in0=r[:, c, :], scalar=ALPHA, in1=pb[j][:, :TT], op0=ALU.mult, op1=ALU.add))
                                    else:
                                        P.op("dve", [r, pb[j]], [r], lambda e, c=c, j=j: e.tensor_tensor(
                                            out=r[:, c, :], in0=r[:, c, :], in1=pb[j][:, :TT], op=ALU.add))
                        items.append((halves, comp))
                run_stream(items)
            emit_layernorm(P, C, r, TT, lambda c: lnp[:, 2 * KC + c:2 * KC + c + 1], lambda c: lnp[:, 3 * KC + c:3 * KC + c + 1],
                           ps[4], ps[5], sq, stat, None)
            P.dma("sp", otok, out_v[:, :, tsl], r[:, :, :], [r], [])
        P.barrier()
    P.cur_es = P.es


def build_F(NT, KM):
    nc = bass.Bass("TRN2", target_bir_lowering=False)
    dr = lambda n, s, d=F32, k="ExternalInput": nc.dram_tensor(n, list(s), d, kind=k).ap()
    io = {"ymix": dr("ymix", [KM, NT], BF16), "xres": dr("xres", [D, NT]), "w_out": dr("w_out", [KM, D]),
          "lnp": dr("lnp", [128, 4 * KC]), "rw": dr("rw", [128, KC * NE]), "rb": dr("rb", [128, NE]),
          "w1": dr("w1", [NE, D, 2 * DEXP]), "b1t": dr("b1t", [128, NE * 8]), "w2": dr("w2", [NE, DEXP, D]),
          "b2": dr("b2", [NE, D]), "ident": dr("ident", [128, 128]), "out": dr("out", [D, NT], F32, "ExternalOutput")}
    with ExitStack() as es:
        P = Prog(nc, es)
        ps = P.make_psum()
        emit_F(nc, P, ps, NT, KM, io)
        P.finish("sp")
    return nc


def _fm(a):
    return np.ascontiguousarray(a.reshape(-1, 128).T)


def host_inputs_F(ymixT, xresT, w_out, g1, b1_, g2, b2_, router_w, router_b, w1, b1, w2, b2):
    lnp = np.concatenate([_fm(g1), _fm(b1_), _fm(g2), _fm(b2_)], axis=1).astype(np.float32)
    rw = np.ascontiguousarray(router_w.reshape(KC, 128, NE).transpose(1, 0, 2).reshape(128, KC * NE))
    rb = np.ascontiguousarray(np.broadcast_to(router_b[None, :], (128, NE)))
    b1t = np.ascontiguousarray(b1.reshape(NE, 8, 128).transpose(2, 0, 1).reshape(128, NE * 8))
    return {
        "ymix": ymixT, "xres": xresT, "w_out": w_out, "lnp": lnp, "rw": rw, "rb": rb,
        "w1": w1, "b1t": b1t, "w2": w2, "b2": b2,
        "ident": np.eye(128, dtype=np.float32),
    }


def emit_C(nc, P, ps, S, io, NHEAD=4):
    TT = 512
    TP = 256
    NT = S // TT
    NP = S // TP
    NB = S // 128
    xT, wc, wq, cw_d, mask_d, tri_d = (io[k] for k in ("xT", "wc", "wq", "convw", "mask", "tri"))
    y_conv, y_attn = io["y_conv"], io["y_attn"]

    with ExitStack() as es:
        P.cur_es = es
        wres = P.sbuf("wres", [128, KC, 1536], BF16)
        xb_t = [P.sbuf(f"xb{i}", [128, KC, TP], BF16).t for i in range(2)]
        xb = [[Buf(f"xb{i}_{h}", xb_t[i]) for h in range(4)] for i in range(2)]
        xtok = [[P.token(f"xt{i}_{h}") for h in range(4)] for i in range(2)]
        cw = P.sbuf("cw", [128, 12], F32)
        mask = P.sbuf("mask_s", [128, 4, 512], F32)
        tri = P.sbuf("tri_s", [128, 256], BF16)
        ctok = P.token("const")
        wtok = [P.token(f"wtk{i}") for i in range(6)]
        otok = [P.token("o0"), P.token("o1")]
        P.dma("sp", ctok, cw[:, :], cw_d[:, :], [], [cw])
        P.dma("sp", ctok, mask[:, :, :], mask_d.rearrange("p (i t) -> p i t", i=4), [], [mask])
        P.dma("sp", ctok, tri[:, :], tri_d[:, :], [], [tri])
        xT_v = xT.rearrange("(c p) t -> p c t", p=128)
        wc_v = wc.rearrange("(c p) f -> p c f", p=128)
        wq_v = wq.rearrange("(c p) f -> p c f", p=128)
        xi = [0]

        def xload(tt):
            i = xi[0] % 2
            xi[0] += 1
            for h in range(4):
                P.dma("pool", xtok[i][h], xb_t[i][:, h * 8:(h + 1) * 8, :], xT_v[:, h * 8:(h + 1) * 8, tt * TP:(tt + 1) * TP], [], [xb[i][h]])
            return xb_t[i], xb[i]

        for q in range(6):
            P.dma("pool", wtok[q], wres[:, :, q * 256:(q + 1) * 256], wc_v[:, :, q * 256:(q + 1) * 256], [], [wres])
        pbuf = P.sbuf("pbuf", [128, 4, TP + 2], F32)
        cgs = [P.sbuf(f"cgs{i}", [128, TP], F32) for i in range(2)]
        acc = [P.sbuf(f"cacc{i}", [128, TP], F32) for i in range(2)]
        yt = [P.sbuf(f"yt{i}", [128, 4, TP], BF16) for i in range(2)]
        P.op("dve", [], [pbuf], lambda e: e.memset(pbuf[:, :, :], 0.0))
        nxt = xload(0)
        for tt in range(NP):
            xt_, xbufs = nxt
            if tt + 1 < NP:
                nxt = xload(tt + 1)
            y = yt[tt % 2]
            for c in range(4):
                pa, pb, pc = ps[(c % 2) * 3], ps[(c % 2) * 3 + 1], ps[(c % 2) * 3 + 2]
                for (pp, col) in ((pa, 512 + c * 128), (pb, 1024 + c * 128), (pc, c * 128)):
                    for k in range(KC):
                        P.op("pe", xbufs + [wres], [pp], lambda e, pp=pp, col=col, k=k: _mm(
                            e, pp[:, :TP], wres[:, k, col:col + 128], xt_[:, k, :], k == 0, k == KC - 1))
                g_, a_ = cgs[c % 2], acc[c % 2]
                P.op("act", [pa], [g_], lambda e, g_=g_, pa=pa: e.copy(out=g_[:, :], in_=pa[:, :TP]))
                P.op("dve", [g_, pb], [pbuf], lambda e, g_=g_, pb=pb, c=c: e.tensor_tensor(out=pbuf[:, c, 2:TP + 2], in0=g_[:, :], in1=pb[:, :TP], op=ALU.mult))
                P.op("dve", [pbuf, cw], [a_], lambda e, a_=a_, c=c: e.tensor_scalar(out=a_[:, :], in0=pbuf[:, c, 0:TP], scalar1=cw[:, c:c + 1], scalar2=None, op0=ALU.mult))
                P.op("dve", [pbuf, cw, a_], [a_], lambda e, a_=a_, c=c: e.scalar_tensor_tensor(out=a_[:, :], in0=pbuf[:, c, 1:TP + 1], scalar=cw[:, 4 + c:5 + c], in1=a_[:, :], op0=ALU.mult, op1=ALU.add))
                P.op("dve", [pbuf, cw, a_], [a_], lambda e, a_=a_, c=c: e.scalar_tensor_tensor(out=a_[:, :], in0=pbuf[:, c, 2:TP + 2], scalar=cw[:, 8 + c:9 + c], in1=a_[:, :], op0=ALU.mult, op1=ALU.add))
                P.op("dve", [a_, pc], [y], lambda e, a_=a_, pc=pc, c=c, y=y: e.tensor_tensor(out=y[:, c, :], in0=a_[:, :], in1=pc[:, :TP], op=ALU.mult))
                P.op("dve", [pbuf], [pbuf], lambda e, c=c: e.tensor_copy(out=pbuf[:, c, 0:2], in_=pbuf[:, c, TP:TP + 2]))
            P.dma("sp", otok[tt % 2], y_conv[:, tt * TP:(tt + 1) * TP].rearrange("(c p) t -> p c t", p=128), y[:, :, :], [y], [])

        P.barrier()
        wres_t = wres.t
        wres = Buf("wres2", wres_t[:, 0:8, :].rearrange("p k (a f) -> p (k a) f", f=384))
        flat = lambda lo, hi: wres_t[:, lo:hi, :].rearrange("p k f -> p (k f)")
        QT = Buf("QT", flat(8, 14)[:, 0:S])
        KT = Buf("KT", flat(14, 20)[:, 0:S])
        V = Buf("V", flat(20, 26)[:, 0:NB * 128].rearrange("p (b d) -> p b d", d=128))
        NS = 2
        eb = [[P.sbuf(f"eb{s_}_{i}", [128, TT], F32) for i in range(2)] for s_ in range(NS)]
        spb = [[P.sbuf(f"spb{s_}_{i}", [128, TT], BF16) for i in range(2)] for s_ in range(NS)]
        exb = [[P.sbuf(f"exb{s_}_{i}", [128, TT], F32) for i in range(2)] for s_ in range(NS)]
        wbf = [[P.sbuf(f"wbf{s_}_{i}", [128, TT], BF16) for i in range(2)] for s_ in range(NS)]
        yo = [[P.sbuf(f"yo{s_}_{i}", [128, TT], BF16) for i in range(2)] for s_ in range(NS)]
        pz = [[ps[0], ps[1]], [ps[4], ps[5]]]
        pacc, po = [ps[2], ps[6]], [ps[3], ps[7]]
        otk = [[P.token(f"oa{s_}_{i}") for i in range(2)] for s_ in range(NS)]
        scale = 128.0 ** -0.5
        for h in range(NHEAD):
            for q in range(3):
                P.dma("pool", wtok[q], wres[:, :, q * 128:(q + 1) * 128], wq_v[:, :, q * 512 + h * 128:q * 512 + (h + 1) * 128], [], [wres])
            nxt = xload(0)
            for tt in range(NP):
                xt_, xbufs = nxt
                if tt + 1 < NP:
                    nxt = xload(tt + 1)
                for (dst, col, pp) in ((QT, 0, ps[4]), (KT, 128, ps[5])):
                    for k in range(KC):
                        P.op("pe", xbufs + [wres], [pp], lambda e, pp=pp, col=col, k=k: _mm(
                            e, pp[:, :TP], wres[:, k, col:col + 128], xt_[:, k, :], k == 0, k == KC - 1))
                    P.op("act", [pp], [dst], lambda e, pp=pp, dst=dst, tt=tt: e.copy(out=dst[:, tt * TP:(tt + 1) * TP], in_=pp[:, :TP]))
                pv = ps[6 + tt % 2]
                for s in range(TP // 128):
                    for k in range(KC):
                        P.op("pe", xbufs + [wres], [pv], lambda e, pv=pv, s=s, k=k: _mm(
                            e, pv[:, s * 128:(s + 1) * 128], xt_[:, k, s * 128:(s + 1) * 128], wres[:, k, 256:384], k == 0, k == KC - 1))
                P.op("dve", [pv], [V], lambda e, pv=pv, tt=tt: e.tensor_copy(out=V[:, tt * (TP // 128):(tt + 1) * (TP // 128), :], in_=pv[:, :TP].rearrange("p (s d) -> p s d", d=128)))
            steps = [[(g, kb) for g in range(NT) if g % NS == s_ for kb in range(4 * g + 3, -1, -1)] for s_ in range(NS)]

            def stageA(s_, n):
                g, kb = steps[s_][n]
                z, e_, sp_ = pz[s_][n % 2], eb[s_][n % 2], spb[s_][n % 2]
                P.op("pe", [KT, QT], [z], lambda e: _mm(e, z[:, :], KT[:, kb * 128:(kb + 1) * 128], QT[:, g * TT:(g + 1) * TT], True, True))
                P.op("act", [z], [e_], lambda e: e.activation(out=e_[:, :], in_=z[:, :], func=AF.Exp, scale=scale))
                P.op("act", [e_], [sp_], lambda e: e.activation(out=sp_[:, :], in_=e_[:, :], func=AF.Ln, bias=1.0))
                i = kb - 4 * g
                if i >= 0:
                    P.op("pool", [sp_, mask], [sp_], lambda e: e.tensor_tensor(out=sp_[:, :], in0=sp_[:, :], in1=mask[:, i, :], op=ALU.mult))
                    P.op("pool", [e_, mask], [e_], lambda e: e.tensor_tensor(out=e_[:, :], in0=e_[:, :], in1=mask[:, i, :], op=ALU.mult))

            def stageB(s_, n):
                g, kb = steps[s_][n]
                e_, sp_, ex_, w_ = eb[s_][n % 2], spb[s_][n % 2], exb[s_][n % 2], wbf[s_][n % 2]
                pa_, po_ = pacc[s_], po[s_]
                first, last = kb == 4 * g + 3, kb == 0
                P.op("pe", [tri, sp_], [pa_], lambda e: _mm(e, pa_[:, :], tri[:, 0:128], sp_[:, :], first, False))
                P.op("act", [pa_], [ex_], lambda e: e.activation(out=ex_[:, :], in_=pa_[:, :], func=AF.Exp))
                P.op("pe", [tri, sp_], [pa_], lambda e: _mm(e, pa_[:, :], tri[:, 128:256], sp_[:, :], False, last))
                P.op("dve", [e_, ex_], [w_], lambda e: e.tensor_tensor(out=w_[:, :], in0=e_[:, :], in1=ex_[:, :], op=ALU.mult))
                P.op("pe", [V, w_], [po_], lambda e: _mm(e, po_[:, :], V[:, kb, :], w_[:, :], first, last))
                if last:
                    y = yo[s_][(g // NS) % 2]
                    P.op("act", [po_], [y], lambda e: e.copy(out=y[:, :], in_=po_[:, :]))
                    P.dma("sp", otk[s_][(g // NS) % 2], y_attn[h * 128:(h + 1) * 128, g * TT:(g + 1) * TT], y[:, :], [y], [])

            live = [s_ for s_ in range(NS) if steps[s_]]
            for s_ in live:
                stageA(s_, 0)
            for n in range(max(len(st) for st in steps)):
                for s_ in live:
                    if n + 1 < len(steps[s_]):
                        stageA(s_, n + 1)
                for s_ in live:
                    if n < len(steps[s_]):
                        stageB(s_, n)
        P.barrier()
    P.cur_es = P.es


def build_C(S):
    nc = bass.Bass("TRN2", target_bir_lowering=False)
    dr = lambda n, s, d=F32, k="ExternalInput": nc.dram_tensor(n, list(s), d, kind=k).ap()
    ymix = dr("ymix", [1024, S], BF16, "ExternalOutput")
    io = {"xT": dr("xT", [D, S]), "wc": dr("wc", [D, 1536]), "wq": dr("wq", [D, 1536]), "convw": dr("convw", [128, 12]),
          "mask": dr("mask", [128, 4 * 512]), "tri": dr("tri", [128, 256], BF16), "y_conv": ymix[0:512, :], "y_attn": ymix[512:1024, :]}
    with ExitStack() as es:
        P = Prog(nc, es)
        ps = P.make_psum()
        emit_C(nc, P, ps, S, io)
        P.finish("sp")
    return nc


def host_consts_C():
    import ml_dtypes
    s = np.arange(128)[:, None]
    t = np.arange(512)[None, :]
    mask = np.stack([(t > 128 * i + s).astype(np.float32) for i in range(4)], axis=1)
    j = np.arange(128)[:, None]
    s2 = np.arange(128)[None, :]
    tri = np.concatenate([-(j >= s2).astype(np.float32), -(j < s2).astype(np.float32)], axis=1)
    return {"mask": np.ascontiguousarray(mask.reshape(128, 2048)), "tri": tri.astype(ml_dtypes.bfloat16)}


def host_inputs_C(x1T_b, w_in, conv_w, hq):
    c0 = hq * 512
    wc = np.concatenate([w_in[:, c0:c0 + 512], w_in[:, 2048 + c0:2048 + c0 + 512], w_in[:, 4096 + c0:4096 + c0 + 512]], axis=1)
    wq = np.concatenate([w_in[:, 6144 + c0:6144 + c0 + 512], w_in[:, 8192 + c0:8192 + c0 + 512], w_in[:, 10240 + c0:10240 + c0 + 512]], axis=1)
    cw = conv_w[:, c0:c0 + 512].reshape(3, 4, 128).transpose(2, 0, 1).reshape(128, 12)
    d = {"xT": x1T_b, "wc": np.ascontiguousarray(wc), "wq": np.ascontiguousarray(wq), "convw": np.ascontiguousarray(cw)}
    d.update(host_consts_C())
    return d


NHG = 12
HP = 64
GC = NHG * HP
WS_COLS = 2 * GC + 256 + NHG


def emit_A_ssd(nc, P, ps, NTOK, SB, io):
    TP = 256
    NTILE = NTOK // TP
    xT, wssd, cwb_d, dtp_d, dsk_d, nw_d, ident_d, ut_d, lt_d, yssd = (
        io[k] for k in ("xT", "wssd", "cwb", "dtp", "dskip", "normw", "ident", "ut", "lt", "yssd"))

    with ExitStack() as es:
        P.cur_es = es
        wres = P.sbuf("wres", [128, KC, WS_COLS], BF16)
        xb_t = [P.sbuf(f"xb{i}", [128, KC, TP], BF16).t for i in range(2)]
        xb = [[Buf(f"xb{i}_{h}", xb_t[i]) for h in range(4)] for i in range(2)]
        xtok = [[P.token(f"xt{i}_{h}") for h in range(4)] for i in range(2)]
        sb = lambda n, s, d=F32: P.sbuf(n, s, d)
        cwb = sb("cwb_s", [128, 8, 5]); dtp = sb("dtp_s", [128, 2 * NHG]); dsk = sb("dsk_s", [128, GC])
        nw = sb("nw_s", [128, GC]); ident = sb("ident_s", [128, 128]); ut = sb("ut_s", [128, 128]); lt = sb("lt_s", [128, 128])
        ones_f = sb("ones_f", [128, 128]); a_bc = sb("a_bc", [128, NHG])
        xpre = sb("xpre", [128, 8, TP + 3]); cacc = [sb(f"cacc{i}", [128, TP]) for i in range(2)]
        xc = sb("xc", [128, 8, TP]); bc16 = sb("bc16", [128, 2, TP], BF16)
        zs = sb("zs", [128, GC]); xtk = sb("xtk", [128, GC]); xdt = sb("xdt", [128, GC], BF16); xdt2 = sb("xdt2", [128, GC], BF16)
        btk = sb("btk", [128, 128], BF16); sml = sb("sml", [128, 8, NHG]); cbm = sb("cbm", [128, 128])
        lh = [sb(f"lh{i}", [128, 128]) for i in range(2)]; dec = [sb(f"dec{i}", [128, 128]) for i in range(2)]
        mh = [sb(f"mh{i}", [128, 128], BF16) for i in range(2)]
        St = sb("St", [128, GC]); S16 = sb("S16", [128, GC], BF16)
        yb = sb("yb", [128, GC]); ytmp = sb("ytmp", [128, GC]); yo = [sb(f"yo{i}", [128, 6, TP], BF16) for i in range(2)]
        ss = sb("ss", [128, 4])
        ctok = P.token("const")
        wtok = [P.token(f"wtk{i}") for i in range(8)]
        otok = [P.token("o0"), P.token("o1")]
        for dst, src in ((dtp, dtp_d), (dsk, dsk_d), (nw, nw_d), (ident, ident_d), (ut, ut_d), (lt, lt_d)):
            P.dma("sp", ctok, dst[:, :], src[:, :], [], [dst])
        P.dma("sp", ctok, cwb[:, :, :], cwb_d.rearrange("p (c i) -> p c i", i=5), [], [cwb])
        P.op("dve", [], [ones_f], lambda e: e.memset(ones_f[:, :], 1.0))
        P.op("act", [dtp], [a_bc], lambda e: e.activation(out=a_bc[:, :], in_=dtp[:, NHG:2 * NHG], func=AF.Exp))
        P.op("dve", [a_bc], [a_bc], lambda e: e.tensor_scalar(out=a_bc[:, :], in0=a_bc[:, :], scalar1=-1.0, scalar2=None, op0=ALU.mult))
        xT_v = xT.rearrange("(c p) t -> p c t", p=128)
        ws_v = wssd.rearrange("(c p) f -> p c f", p=128)
        xi = [0]

        def xload(src_v, tt):
            i = xi[0] % 2
            xi[0] += 1
            for h in range(4):
                P.dma("pool", xtok[i][h], xb_t[i][:, h * 8:(h + 1) * 8, :], src_v[:, h * 8:(h + 1) * 8, tt * TP:(tt + 1) * TP], [], [xb[i][h]])
            return xb_t[i], xb[i]

        bounds = [0, 256, 512, 768, 1024, 1280, 1536, 1792, WS_COLS]
        for q in range(8):
            P.dma("pool", wtok[q], wres[:, :, bounds[q]:bounds[q + 1]], ws_v[:, :, bounds[q]:bounds[q + 1]], [], [wres])

        bc3 = lambda ap: ap.unsqueeze(2).to_broadcast([128, ap.shape[1], HP])
        v3 = lambda ap: ap.rearrange("p (h d) -> p h d", d=HP)
        dt_t, da_t, ac_t, te_t, ea_t, et_t, dtt_t, tmp_t = (sml[:, i, :] for i in range(8))
        XO, BO, CO, DTO = GC, 2 * GC, 2 * GC + 128, 2 * GC + 256

        nxt = xload(xT_v, 0)
        for tt in range(NTILE):
            xt_, xbufs = nxt
            if tt + 1 < NTILE:
                nxt = xload(xT_v, tt + 1)
            if (tt * TP) % SB == 0:
                P.op("dve", [], [xpre], lambda e: e.memset(xpre[:, :, 0:3], 0.0))
                P.op("dve", [], [St], lambda e: e.memset(St[:, :], 0.0))
                P.op("dve", [], [S16], lambda e: e.memset(S16[:, :], 0.0))
            for ch in range(8):
                col = XO + ch * 128 if ch < 6 else (BO if ch == 6 else CO)
                pp = ps[ch % 2]
                for k in range(KC):
                    P.op("pe", xbufs + [wres], [pp], lambda e, pp=pp, col=col, k=k: _mm(
                        e, pp[:, :TP], wres[:, k, col:col + 128], xt_[:, k, :], k == 0, k == KC - 1))
                P.op("act", [pp], [xpre], lambda e, pp=pp, ch=ch: e.copy(out=xpre[:, ch, 3:TP + 3], in_=pp[:, :TP]))
                a_ = cacc[ch % 2]
                P.op("dve", [xpre, cwb], [a_], lambda e, a_=a_, ch=ch: e.tensor_scalar(out=a_[:, :], in0=xpre[:, ch, 0:TP], scalar1=cwb[:, ch, 0:1], scalar2=None, op0=ALU.mult))
                for i in range(1, 4):
                    P.op("dve", [xpre, cwb, a_], [a_], lambda e, a_=a_, ch=ch, i=i: e.scalar_tensor_tensor(
                        out=a_[:, :], in0=xpre[:, ch, i:TP + i], scalar=cwb[:, ch, i:i + 1], in1=a_[:, :], op0=ALU.mult, op1=ALU.add))
                P.op("act", [a_, cwb], [xc], lambda e, a_=a_, ch=ch: e.activation(out=xc[:, ch, :], in_=a_[:, :], func=AF.Silu, bias=cwb[:, ch, 4:5], scale=1.0))
                P.op("dve", [xpre], [xpre], lambda e, ch=ch: e.tensor_copy(out=xpre[:, ch, 0:3], in_=xpre[:, ch, TP:TP + 3]))
            P.op("pool", [xc], [bc16], lambda e: e.tensor_copy(out=bc16[:, :, :], in_=xc[:, 6:8, :]))
            for j in range(TP // 128):
                csl = slice(j * 128, (j + 1) * 128)
                tok0 = tt * TP + j * 128
                for k in range(KC):
                    P.op("pe", xbufs + [wres], [ps[2]], lambda e, k=k: _mm(e, ps[2][:, 0:512], xt_[:, k, csl], wres[:, k, 0:512], k == 0, k == KC - 1))
                for k in range(KC):
                    P.op("pe", xbufs + [wres], [ps[3]], lambda e, k=k: _mm(e, ps[3][:, 0:256], xt_[:, k, csl], wres[:, k, 512:768], k == 0, k == KC - 1))
                for k in range(KC):
                    P.op("pe", xbufs + [wres], [ps[3]], lambda e, k=k: _mm(e, ps[3][:, 256:256 + NHG], xt_[:, k, csl], wres[:, k, DTO:DTO + NHG], k == 0, k == KC - 1))
                P.op("act", [ps[2]], [zs], lambda e: e.activation(out=zs[:, 0:512], in_=ps[2][:, 0:512], func=AF.Silu))
                P.op("act", [ps[3]], [zs], lambda e: e.activation(out=zs[:, 512:768], in_=ps[3][:, 0:256], func=AF.Silu))
                P.op("dve", [ps[3], dtp], [sml], lambda e: e.tensor_tensor(out=dt_t, in0=ps[3][:, 256:256 + NHG], in1=dtp[:, 0:NHG], op=ALU.add))
                P.op("act", [sml], [sml], lambda e: e.activation(out=dt_t, in_=dt_t, func=AF.Exp))
                P.op("act", [sml], [sml], lambda e: e.activation(out=dt_t, in_=dt_t, func=AF.Ln, bias=1.0))
                P.op("dve", [sml, a_bc], [sml], lambda e: e.tensor_tensor(out=da_t, in0=dt_t, in1=a_bc[:, :], op=ALU.mult))
                for ch in range(6):
                    pb_, off = (ps[4], ch * 128) if ch < 4 else (ps[5], (ch - 4) * 128)
                    P.op("pe", [xc, ident], [pb_], lambda e, pb_=pb_, off=off, ch=ch: e.transpose(pb_[:, off:off + 128], xc[:, ch, csl], ident[:, :]))
                P.op("pe", [xc, ident], [ps[5]], lambda e: e.transpose(ps[5][:, 256:384], xc[:, 6, csl], ident[:, :]))
                P.op("act", [ps[4]], [xtk], lambda e: e.copy(out=xtk[:, 0:512], in_=ps[4][:, 0:512]))
                P.op("act", [ps[5]], [xtk], lambda e: e.copy(out=xtk[:, 512:768], in_=ps[5][:, 0:256]))
                P.op("dve", [ps[5]], [btk], lambda e: e.tensor_copy(out=btk[:, :], in_=ps[5][:, 256:384]))
                P.op("pe", [lt, sml], [ps[3]], lambda e: _mm(e, ps[3][:, 272:272 + NHG], lt[:, :], da_t, True, True))
                P.op("pe", [ones_f, sml], [ps[3]], lambda e: _mm(e, ps[3][:, 288:288 + NHG], ones_f[:, :], da_t, True, True))
                P.op("pe", [bc16], [ps[3]], lambda e: _mm(e, ps[3][:, 384:512], bc16[:, 0, csl], bc16[:, 1, csl], True, True))
                P.op("dve", [ps[3]], [sml], lambda e: e.tensor_copy(out=ac_t, in_=ps[3][:, 272:272 + NHG]))
                P.op("dve", [ps[3], sml], [sml], lambda e: e.tensor_tensor(out=te_t, in0=ps[3][:, 288:288 + NHG], in1=ac_t, op=ALU.subtract))
                P.op("act", [sml], [sml], lambda e: e.activation(out=te_t, in_=te_t, func=AF.Exp))
                P.op("act", [sml], [sml], lambda e: e.activation(out=ea_t, in_=ac_t, func=AF.Exp))
                P.op("act", [ps[3]], [sml], lambda e: e.activation(out=et_t, in_=ps[3][:, 288:288 + NHG], func=AF.Exp))
                P.op("dve", [sml], [sml], lambda e: e.tensor_tensor(out=dtt_t, in0=dt_t, in1=te_t, op=ALU.mult))
                P.op("dve", [ps[3], lt], [cbm], lambda e: e.tensor_tensor(out=cbm[:, :], in0=ps[3][:, 384:512], in1=lt[:, :], op=ALU.mult))
                P.op("dve", [xtk, sml], [xdt], lambda e: e.tensor_tensor(out=v3(xdt[:, :]), in0=v3(xtk[:, :]), in1=bc3(dt_t), op=ALU.mult))
                P.op("dve", [xtk, sml], [xdt2], lambda e: e.tensor_tensor(out=v3(xdt2[:, :]), in0=v3(xtk[:, :]), in1=bc3(dtt_t), op=ALU.mult))
                P.op("pe", [bc16, S16], [ps[6]], lambda e: _mm(e, ps[6][:, 0:512], bc16[:, 1, csl], S16[:, 0:512], True, True))
                P.op("pe", [bc16, S16], [ps[7]], lambda e: _mm(e, ps[7][:, 0:256], bc16[:, 1, csl], S16[:, 512:768], True, True))
                P.op("pe", [btk, xdt2], [ps[2]], lambda e: _mm(e, ps[2][:, 0:512], btk[:, :], xdt2[:, 0:512], True, True))
                P.op("pe", [btk, xdt2], [ps[3]], lambda e: _mm(e, ps[3][:, 0:256], btk[:, :], xdt2[:, 512:768], True, True))
                for h in range(NHG):
                    l_, d_, m_ = lh[h % 2], dec[h % 2], mh[h % 2]
                    pseg = ps[h % 2]
                    P.op("dve", [ut, sml], [l_], lambda e, l_=l_, h=h: e.tensor_scalar(out=l_[:, :], in0=ut[:, :], scalar1=da_t[:, h:h + 1], scalar2=None, op0=ALU.mult))
                    P.op("pe", [l_, lt], [pseg], lambda e, l_=l_, pseg=pseg: _mm(e, pseg[:, 0:128], l_[:, :], lt[:, :], True, True))
                    P.op("act", [pseg], [d_], lambda e, d_=d_, pseg=pseg: e.activation(out=d_[:, :], in_=pseg[:, 0:128], func=AF.Exp))
                    P.op("dve", [d_, cbm], [m_], lambda e, d_=d_, m_=m_: e.tensor_tensor(out=m_[:, :], in0=d_[:, :], in1=cbm[:, :], op=ALU.mult))
                    py, off = (ps[4], h * HP) if h < 8 else (ps[5], (h - 8) * HP)
                    P.op("pe", [m_, xdt], [py], lambda e, m_=m_, py=py, off=off, h=h: _mm(e, py[:, off:off + HP], m_[:, :], xdt[:, h * HP:(h + 1) * HP], True, True))
                P.op("dve", [ps[6], sml], [yb], lambda e: e.tensor_tensor(out=v3(yb[:, 0:512]), in0=v3(ps[6][:, 0:512]), in1=bc3(ea_t[:, 0:8]), op=ALU.mult))
                P.op("dve", [ps[7], sml], [yb], lambda e: e.tensor_tensor(out=v3(yb[:, 512:768]), in0=v3(ps[7][:, 0:256]), in1=bc3(ea_t[:, 8:12]), op=ALU.mult))
                P.op("dve", [ps[4], yb], [yb], lambda e: e.tensor_tensor(out=yb[:, 0:512], in0=yb[:, 0:512], in1=ps[4][:, 0:512], op=ALU.add))
                P.op("dve", [ps[5], yb], [yb], lambda e: e.tensor_tensor(out=yb[:, 512:768], in0=yb[:, 512:768], in1=ps[5][:, 0:256], op=ALU.add))
                P.op("pool", [xtk, dsk], [ytmp], lambda e: e.tensor_tensor(out=ytmp[:, :], in0=xtk[:, :], in1=dsk[:, :], op=ALU.mult))
                P.op("dve", [yb, ytmp], [yb], lambda e: e.tensor_tensor(out=yb[:, :], in0=yb[:, :], in1=ytmp[:, :], op=ALU.add))
                P.op("dve", [yb, zs], [yb], lambda e: e.tensor_tensor(out=yb[:, :], in0=yb[:, :], in1=zs[:, :], op=ALU.mult))
                P.op("pool", [yb], [ytmp], lambda e: e.tensor_tensor(out=ytmp[:, :], in0=yb[:, :], in1=yb[:, :], op=ALU.mult))
                P.op("dve", [ytmp], [ss], lambda e: e.reduce_sum(out=ss[:, 0:1], in_=ytmp[:, :], axis=AX.X))
                P.op("dve", [ss], [ss], lambda e: e.tensor_scalar(out=ss[:, 1:2], in0=ss[:, 0:1], scalar1=1.0 / GC, scalar2=RMS_EPS, op0=ALU.mult, op1=ALU.add))
                P.op("act", [ss], [ss], lambda e: e.activation(out=ss[:, 2:3], in_=ss[:, 1:2], func=AF.Ln))
                P.op("act", [ss], [ss], lambda e: e.activation(out=ss[:, 3:4], in_=ss[:, 2:3], func=AF.Exp, scale=-0.5))
                P.op("dve", [yb, ss, nw], [ytmp], lambda e: e.scalar_tensor_tensor(out=ytmp[:, :], in0=yb[:, :], scalar=ss[:, 3:4], in1=nw[:, :], op0=ALU.mult, op1=ALU.mult))
                yf = yo[tt % 2]
                for ch in range(6):
                    pb_, off = (ps[4], ch * 128) if ch < 4 else (ps[5], (ch - 4) * 128)
                    P.op("pe", [ytmp, ident], [pb_], lambda e, pb_=pb_, off=off, ch=ch: e.transpose(pb_[:, off:off + 128], ytmp[:, ch * 128:(ch + 1) * 128], ident[:, :]))
                P.op("act", [ps[4]], [yf], lambda e, yf=yf: e.copy(out=yf[:, 0:4, csl], in_=ps[4][:, 0:512].rearrange("p (c t) -> p c t", t=128)))
                P.op("act", [ps[5]], [yf], lambda e, yf=yf: e.copy(out=yf[:, 4:6, csl], in_=ps[5][:, 0:256].rearrange("p (c t) -> p c t", t=128)))
                if j == TP // 128 - 1:
                    P.dma("sp", otok[tt % 2], yssd[:, tt * TP:(tt + 1) * TP].rearrange("(c p) t -> p c t", p=128), yf[:, :, :], [yf], [])
                P.op("dve", [St, sml], [St], lambda e: e.tensor_tensor(out=v3(St[:, :]), in0=v3(St[:, :]), in1=bc3(et_t), op=ALU.mult))
                P.op("dve", [St, ps[2]], [St], lambda e: e.tensor_tensor(out=St[:, 0:512], in0=St[:, 0:512], in1=ps[2][:, 0:512], op=ALU.add))
                P.op("dve", [St, ps[3]], [St], lambda e: e.tensor_tensor(out=St[:, 512:768], in0=St[:, 512:768], in1=ps[3][:, 0:256], op=ALU.add))
                P.op("pool", [St], [S16], lambda e: e.tensor_copy(out=S16[:, :], in_=St[:, :]))

        P.barrier()
    P.cur_es = P.es


def emit_A_pool(nc, P, ps, SB, io):
    TP = 256
    xTp, wpool, pw_d, psc_d, psel_d, icnt_d, ypool = (io[k] for k in ("xTp", "wpool", "poolw", "poolsc", "psel", "invcnt0", "ypool"))
    with ExitStack() as es:
        P.cur_es = es
        sb = lambda n, s, d=F32: P.sbuf(n, s, d)
        xb_t = [P.sbuf(f"xb{i}", [128, KC, TP], BF16).t for i in range(2)]
        xb = [[Buf(f"xb{i}_{h}", xb_t[i]) for h in range(4)] for i in range(2)]
        xtok = [[P.token(f"xt{i}_{h}") for h in range(4)] for i in range(2)]
        ctok = P.token("const")
        wtok = [P.token(f"wtk{i}") for i in range(8)]
        otok = [P.token("o0"), P.token("o1")]
        xTp_v = xTp.rearrange("(c p) t -> p c t", p=128)
        wp_v = wpool.rearrange("(c p) f -> p c f", p=128)
        xi = [0]

        def xload(src_v, tt):
            i = xi[0] % 2
            xi[0] += 1
            for h in range(4):
                P.dma("pool", xtok[i][h], xb_t[i][:, h * 8:(h + 1) * 8, :], src_v[:, h * 8:(h + 1) * 8, tt * TP:(tt + 1) * TP], [], [xb[i][h]])
            return xb_t[i], xb[i]

        wp = P.sbuf("wp", [128, KC, 512], BF16)
        pw = P.sbuf("pw", [128, 4, 512], BF16)
        psc = sb("psc_s", [128, 4]); psel = sb("psel_s", [128, 5]); icnt = sb("icnt_s", [128, TP])
        W = TP + 16
        ub = sb("ub", [128, 4, W]); sA = sb("sA", [128, 4, W]); sB_ = sb("sB", [128, 4, W])
        pac = sb("pac", [128, 4, TP]); m16 = sb("m16", [128, 4, TP], BF16); yp = [sb(f"yp{i}", [128, 4, TP], BF16) for i in range(2)]
        for dst, src in ((psc, psc_d), (psel, psel_d), (icnt, icnt_d)):
            P.dma("sp", ctok, dst[:, :], src[:, :], [], [dst])
        P.dma("pool", wtok[0], pw[:, :, :], pw_d.rearrange("(c p) f -> p c f", p=128), [], [pw])
        for q in range(2):
            P.dma("pool", wtok[1 + q], wp[:, :, q * 256:(q + 1) * 256], wp_v[:, :, q * 256:(q + 1) * 256], [], [wp])
        P.op("dve", [], [ub], lambda e: e.memset(ub[:, :, 0:16], 0.0))
        NPT = SB // TP
        nxt = xload(xTp_v, 0)
        for tt in range(NPT):
            xt_, xbufs = nxt
            if tt + 1 < NPT:
                nxt = xload(xTp_v, tt + 1)
            for c in range(4):
                pp = ps[c % 2]
                for k in range(KC):
                    P.op("pe", xbufs + [wp], [pp], lambda e, pp=pp, c=c, k=k: _mm(e, pp[:, :TP], wp[:, k, c * 128:(c + 1) * 128], xt_[:, k, :], k == 0, k == KC - 1))
                P.op("act", [pp], [ub], lambda e, pp=pp, c=c: e.copy(out=ub[:, c, 16:W], in_=pp[:, :TP]))
            P.op("dve", [ub], [sA], lambda e: e.tensor_tensor(out=sA[:, :, 1:W], in0=ub[:, :, 1:W], in1=ub[:, :, 0:W - 1], op=ALU.add))
            P.op("dve", [sA, psel], [pac], lambda e: e.tensor_scalar(out=pac[:, :, :], in0=sA[:, :, 16:W], scalar1=psel[:, 0:1], scalar2=None, op0=ALU.mult))
            P.op("dve", [sA], [sB_], lambda e: e.tensor_tensor(out=sB_[:, :, 3:W], in0=sA[:, :, 3:W], in1=sA[:, :, 1:W - 2], op=ALU.add))
            P.op("dve", [sB_, psel, pac], [pac], lambda e: e.scalar_tensor_tensor(out=pac[:, :, :], in0=sB_[:, :, 16:W], scalar=psel[:, 1:2], in1=pac[:, :, :], op0=ALU.mult, op1=ALU.add))
            P.op("dve", [sB_], [sA], lambda e: e.tensor_tensor(out=sA[:, :, 7:W], in0=sB_[:, :, 7:W], in1=sB_[:, :, 3:W - 4], op=ALU.add))
            P.op("dve", [sA, psel, pac], [pac], lambda e: e.scalar_tensor_tensor(out=pac[:, :, :], in0=sA[:, :, 16:W], scalar=psel[:, 2:3], in1=pac[:, :, :], op0=ALU.mult, op1=ALU.add))
            P.op("dve", [sA], [sB_], lambda e: e.tensor_tensor(out=sB_[:, :, 15:W], in0=sA[:, :, 15:W], in1=sA[:, :, 7:W - 8], op=ALU.add))
            P.op("dve", [sB_, psel, pac], [pac], lambda e: e.scalar_tensor_tensor(out=pac[:, :, :], in0=sB_[:, :, 16:W], scalar=psel[:, 3:4], in1=pac[:, :, :], op0=ALU.mult, op1=ALU.add))
            if tt == 0:
                P.op("dve", [pac, icnt], [pac], lambda e: e.tensor_tensor(out=pac[:, :, :], in0=pac[:, :, :], in1=icnt[:, :].unsqueeze(1).to_broadcast([128, 4, TP]), op=ALU.mult))
                P.op("dve", [pac, ub], [m16], lambda e: e.tensor_tensor(out=m16[:, :, :], in0=pac[:, :, :], in1=ub[:, :, 16:W], op=ALU.subtract))
            else:
                P.op("dve", [pac, ub, psel], [m16], lambda e: e.scalar_tensor_tensor(out=m16[:, :, :], in0=pac[:, :, :], scalar=psel[:, 4:5], in1=ub[:, :, 16:W], op0=ALU.mult, op1=ALU.subtract))
            P.op("dve", [ub], [ub], lambda e: e.tensor_copy(out=ub[:, :, 0:16], in_=ub[:, :, TP:W]))
            y = yp[tt % 2]
            for dch in range(4):
                pp = ps[2 + dch % 2]
                for k in range(4):
                    P.op("pe", [pw, m16], [pp], lambda e, pp=pp, dch=dch, k=k: _mm(e, pp[:, :TP], pw[:, k, dch * 128:(dch + 1) * 128], m16[:, k, :], k == 0, k == 3))
                P.op("act", [pp, psc], [y], lambda e, pp=pp, dch=dch, y=y: e.activation(out=y[:, dch, :], in_=pp[:, :TP], func=AF.Copy, scale=psc[:, dch:dch + 1]))
            P.dma("sp", otok[tt % 2], ypool[:, tt * TP:(tt + 1) * TP].rearrange("(c p) t -> p c t", p=128), y[:, :, :], [y], [])
        P.barrier()
    P.cur_es = P.es


def build_A(SB, NBATCH=2):
    NTOK = NBATCH * SB
    nc = bass.Bass("TRN2", target_bir_lowering=False)
    dr = lambda n, s, d=F32, k="ExternalInput": nc.dram_tensor(n, list(s), d, kind=k).ap()
    io = {"xT": dr("xT", [D, NTOK]), "xTp": dr("xTp", [D, SB]), "wssd": dr("wssd", [D, WS_COLS]), "wpool": dr("wpool", [D, 512]),
          "cwb": dr("cwb", [128, 8 * 5]), "dtp": dr("dtp", [128, 2 * NHG]), "dskip": dr("dskip", [128, GC]), "normw": dr("normw", [128, GC]),
          "poolw": dr("poolw", [512, 512]), "poolsc": dr("poolsc", [128, 4]), "psel": dr("psel", [128, 5]), "invcnt0": dr("invcnt0", [128, 256]),
          "ident": dr("ident", [128, 128]), "ut": dr("ut", [128, 128]), "lt": dr("lt", [128, 128]),
          "yssd": dr("yssd", [GC, NTOK], BF16, "ExternalOutput"), "ypool": dr("ypool", [512, SB], BF16, "ExternalOutput")}
    with ExitStack() as es:
        P = Prog(nc, es)
        ps = P.make_psum()
        emit_A_ssd(nc, P, ps, NTOK, SB, io)
        emit_A_pool(nc, P, ps, SB, io)
        P.finish("sp")
    return nc


POOL_WINDOWS = (2, 4, 8, 16)


def host_inputs_A(xT_full, xT_b, w_in, conv_w, conv_b, dt_bias, a_log, d_skip, norm_w, pool_w, pool_scale, g, pg, TP=256):
    zc = 2048 + g * GC
    xcol = 8192 + g * GC
    bcol = 8192 + 6144 + g * 128
    ccol = 8192 + 6144 + 1024 + g * 128
    dcol = 16384 + g * NHG
    wssd = np.concatenate([w_in[:, zc:zc + GC], w_in[:, xcol:xcol + GC], w_in[:, bcol:bcol + 128], w_in[:, ccol:ccol + 128], w_in[:, dcol:dcol + NHG]], axis=1)
    wpool = w_in[:, pg * 512:(pg + 1) * 512]
    cidx = np.concatenate([np.arange(g * GC, (g + 1) * GC), 6144 + g * 128 + np.arange(128), 6144 + 1024 + g * 128 + np.arange(128)])
    cw = conv_w[:, cidx]
    cb = conv_b[cidx]
    cwb = np.concatenate([cw, cb[None, :]], axis=0)
    cwb = cwb.reshape(5, 8, 128).transpose(2, 1, 0).reshape(128, 40)
    hs = slice(g * NHG, (g + 1) * NHG)
    dtp = np.broadcast_to(np.concatenate([dt_bias[hs], a_log[hs]])[None, :], (128, 2 * NHG))
    dsk = np.broadcast_to(np.repeat(d_skip[hs], HP)[None, :], (128, GC))
    nwb = np.broadcast_to(norm_w[g * GC:(g + 1) * GC][None, :], (128, GC))
    psc = pool_scale[pg * 512:(pg + 1) * 512].reshape(4, 128).T
    w = POOL_WINDOWS[pg]
    psel = np.zeros((128, 5), np.float32)
    psel[:, pg] = 1.0
    psel[:, 4] = 1.0 / w
    pos = np.arange(1, TP + 1, dtype=np.float32)
    icnt = np.broadcast_to((1.0 / np.minimum(pos, float(w)))[None, :], (128, TP))
    k = np.arange(128)
    ca = lambda a: np.ascontiguousarray(a, dtype=np.float32)
    return {
        "xT": xT_full, "xTp": xT_b, "wssd": ca(wssd), "wpool": ca(wpool), "cwb": ca(cwb), "dtp": ca(dtp),
        "dskip": ca(dsk), "normw": ca(nwb), "poolw": ca(pool_w[pg]), "poolsc": ca(psc), "psel": psel, "invcnt0": ca(icnt),
        "ident": np.eye(128, dtype=np.float32), "ut": ca(k[:, None] > k[None, :]), "lt": ca(k[:, None] <= k[None, :]),
    }


def build_fused(S):
    nc = bass.Bass("TRN2", target_bir_lowering=False)
    dr = lambda n, s, d=F32, k="ExternalInput": nc.dram_tensor(n, list(s), d, kind=k).ap()
    xT = dr("xT", [D, S])
    wssd_all = dr("wssd_all", [8, D, WS_COLS]); cwb_all = dr("cwb_all", [8, 128, 40]); dtp_all = dr("dtp_all", [8, 128, 2 * NHG])
    dsk_all = dr("dsk_all", [8, 128, GC]); nw_all = dr("nw_all", [8, 128, GC]); wu = dr("wu", [D, 2048])
    poolw = dr("poolw", [4, 512, 512]); poolsc_all = dr("poolsc_all", [4, 128, 4]); psel_all = dr("psel_all", [4, 128, 5])
    icnt_all = dr("icnt_all", [4, 128, 256])
    ident = dr("ident", [128, 128]); ut = dr("ut", [128, 128]); lt = dr("lt", [128, 128])
    Fio = []
    for l, km in ((0, 8192), (1, 4096)):
        Fio.append({"w_out": dr(f"w_out{l}", [km, D]), "lnp": dr(f"lnp{l}", [128, 4 * KC]), "rw": dr(f"rw{l}", [128, KC * NE]),
                    "rb": dr(f"rb{l}", [128, NE]), "w1": dr(f"w1_{l}", [NE, D, 2 * DEXP]), "b1t": dr(f"b1t{l}", [128, NE * 8]),
                    "w2": dr(f"w2_{l}", [NE, DEXP, D]), "b2": dr(f"b2_{l}", [NE, D]), "ident": ident})
    wc_all = dr("wc_all", [4, D, 1536]); wq_all = dr("wq_all", [4, D, 1536]); convw_all = dr("convw_all", [4, 128, 12])
    mask = dr("mask", [128, 4 * 512]); tri = dr("tri", [128, 256], BF16)
    out = dr("out", [D, S], F32, "ExternalOutput")
    ymix0 = nc.dram_tensor("ymix0_scr", [8192, S], BF16).ap()
    x1 = nc.dram_tensor("x1_scr", [D, S], F32).ap()
    ymix1 = nc.dram_tensor("ymix1_scr", [4096, S], BF16).ap()
    with ExitStack() as es:
        P = Prog(nc, es)
        ps = P.make_psum()
        for g in range(8):
            emit_A_ssd(nc, P, ps, S, S, {"xT": xT, "wssd": wssd_all[g], "cwb": cwb_all[g], "dtp": dtp_all[g], "dskip": dsk_all[g],
                                         "normw": nw_all[g], "ident": ident, "ut": ut, "lt": lt,
                                         "yssd": ymix0[2048 + g * GC:2048 + (g + 1) * GC, :]})
        for pg in range(4):
            emit_A_pool(nc, P, ps, S, {"xTp": xT, "wpool": wu[:, pg * 512:(pg + 1) * 512], "poolw": poolw[pg], "poolsc": poolsc_all[pg],
                                       "psel": psel_all[pg], "invcnt0": icnt_all[pg], "ypool": ymix0[pg * 512:(pg + 1) * 512, :]})
        emit_F(nc, P, ps, S, 8192, dict(Fio[0], ymix=ymix0, xres=xT, out=x1))
        for hq in range(4):
            emit_C(nc, P, ps, S, {"xT": x1, "wc": wc_all[hq], "wq": wq_all[hq], "convw": convw_all[hq], "mask": mask, "tri": tri,
                                  "y_conv": ymix1[hq * 512:(hq + 1) * 512, :], "y_attn": ymix1[2048 + hq * 512:2048 + (hq + 1) * 512, :]})
        emit_F(nc, P, ps, S, 4096, dict(Fio[1], ymix=ymix1, xres=x1, out=out))
        P.finish("sp")
    return nc


def host_inputs_fused(xT_b, p):
    dummy = np.zeros((D, 8), np.float32)
    A = [host_inputs_A(dummy, dummy, p["l0_w_in"], p["l0_conv_w"], p["l0_conv_b"], p["l0_dt_bias"], p["l0_a_log"], p["l0_d_skip"],
                       p["l0_ssm_norm_w"], p["l0_pool_w"], p["l0_pool_scale"], g, g % 4) for g in range(8)]
    st = lambda k, n: np.ascontiguousarray(np.stack([A[i][k] for i in range(n)]))
    d = {"xT": xT_b, "wssd_all": st("wssd", 8), "cwb_all": st("cwb", 8), "dtp_all": st("dtp", 8), "dsk_all": st("dskip", 8),
         "nw_all": st("normw", 8), "wu": np.ascontiguousarray(p["l0_w_in"][:, 0:2048]), "poolw": p["l0_pool_w"],
         "poolsc_all": st("poolsc", 4), "psel_all": st("psel", 4), "icnt_all": st("invcnt0", 4),
         "ident": A[0]["ident"], "ut": A[0]["ut"], "lt": A[0]["lt"]}
    for l in (0, 1):
        q = f"l{l}_"
        F_ = host_inputs_F(None, None, p[q + "w_out"], p[q + "ln_mix_g"], p[q + "ln_mix_b"], p[q + "ln_ffn_g"], p[q + "ln_ffn_b"],
                           p[q + "router_w"], p[q + "router_b"], p[q + "w1"], p[q + "b1"], p[q + "w2"], p[q + "b2"])
        d.update({f"w_out{l}": F_["w_out"], f"lnp{l}": F_["lnp"], f"rw{l}": F_["rw"], f"rb{l}": F_["rb"], f"w1_{l}": F_["w1"],
                  f"b1t{l}": F_["b1t"], f"w2_{l}": F_["w2"], f"b2_{l}": F_["b2"]})
    Cs = [host_inputs_C(None, p["l1_w_in"], p["l1_conv_w"], hq) for hq in range(4)]
    d.update({"wc_all": np.ascontiguousarray(np.stack([c["wc"] for c in Cs])), "wq_all": np.ascontiguousarray(np.stack([c["wq"] for c in Cs])),
              "convw_all": np.ascontiguousarray(np.stack([c["convw"] for c in Cs])), "mask": Cs[0]["mask"], "tri": Cs[0]["tri"]})
    return d


def kernel_fused(**inputs):
    p = {k: np.asarray(v, dtype=np.float32) for k, v in inputs.items()}
    x = p.pop("x")
    B, S, _ = x.shape
    nc = _prog(("fused", S), lambda: build_fused(S))
    maps = []
    base = None
    for b in range(B):
        xT_b = np.ascontiguousarray(x[b].T)
        if base is None:
            base = host_inputs_fused(xT_b, p)
            maps.append(base)
        else:
            m = dict(base)
            m["xT"] = xT_b
            maps.append(m)
    outs = _run(nc, maps)
    return np.ascontiguousarray(np.stack([o["out"].T for o in outs])).astype(np.float32)


NCORES = 8
BATCH = 2
SEQ = 8192
_PROG_CACHE = {}


def _prog(key, fn):
    if key not in _PROG_CACHE:
        _PROG_CACHE[key] = fn()
    return _PROG_CACHE[key]


def _run(nc, in_maps):
    res = run_bass_kernel_spmd(nc, in_maps, core_ids=list(range(len(in_maps))))
    return res.results


def _layer_F(ymixT, xresT, KM, w_out, ln_mix_g, ln_mix_b, ln_ffn_g, ln_ffn_b, router_w, router_b, w1, b1, w2, b2):
    NTOK = xresT.shape[1]
    NT = NTOK // NCORES
    nc = _prog(("F", NT, KM), lambda: build_F(NT, KM))
    maps = []
    for c in range(NCORES):
        sl = slice(c * NT, (c + 1) * NT)
        maps.append(host_inputs_F(np.ascontiguousarray(ymixT[:, sl]), np.ascontiguousarray(xresT[:, sl]), w_out,
                                  ln_mix_g, ln_mix_b, ln_ffn_g, ln_ffn_b, router_w, router_b, w1, b1, w2, b2))
    outs = _run(nc, maps)
    return np.concatenate([o["out"] for o in outs], axis=1)


def kernel_unfused(x,
           l0_w_in, l0_conv_w, l0_conv_b, l0_dt_bias, l0_a_log, l0_d_skip, l0_ssm_norm_w,
           l0_pool_w, l0_pool_scale, l0_w_out, l0_ln_mix_g, l0_ln_mix_b,
           l0_router_w, l0_router_b, l0_w1, l0_b1, l0_w2, l0_b2, l0_ln_ffn_g, l0_ln_ffn_b,
           l1_w_in, l1_conv_w, l1_w_out, l1_ln_mix_g, l1_ln_mix_b,
           l1_router_w, l1_router_b, l1_w1, l1_b1, l1_w2, l1_b2, l1_ln_ffn_g, l1_ln_ffn_b):
    import ml_dtypes
    f32 = lambda a: np.asarray(a, dtype=np.float32)
    x = f32(x)
    B, S, _ = x.shape
    NTOK = B * S
    xT = np.ascontiguousarray(x.reshape(NTOK, D).T)

    ncA = _prog(("A", S), lambda: build_A(S, B))
    w_in0 = f32(l0_w_in)
    maps = []
    for c in range(NCORES):
        b, pg = c // 4, c % 4
        maps.append(host_inputs_A(xT, np.ascontiguousarray(xT[:, b * S:(b + 1) * S]), w_in0, f32(l0_conv_w), f32(l0_conv_b),
                                  f32(l0_dt_bias), f32(l0_a_log), f32(l0_d_skip), f32(l0_ssm_norm_w), f32(l0_pool_w),
                                  f32(l0_pool_scale), c, pg))
    outs = _run(ncA, maps)
    ymix0 = np.empty((8192, NTOK), dtype=ml_dtypes.bfloat16)
    for c in range(NCORES):
        b, pg = c // 4, c % 4
        ymix0[pg * 512:(pg + 1) * 512, b * S:(b + 1) * S] = outs[c]["ypool"]
        ymix0[2048 + c * GC:2048 + (c + 1) * GC, :] = outs[c]["yssd"]
    del outs, maps

    x1T = _layer_F(ymix0, xT, 8192, f32(l0_w_out), f32(l0_ln_mix_g), f32(l0_ln_mix_b), f32(l0_ln_ffn_g), f32(l0_ln_ffn_b),
                   f32(l0_router_w), f32(l0_router_b), f32(l0_w1), f32(l0_b1), f32(l0_w2), f32(l0_b2))
    del ymix0

    ncC = _prog(("C", S), lambda: build_C(S))
    w_in1 = f32(l1_w_in)
    maps = []
    for c in range(NCORES):
        b, hq = c // 4, c % 4
        maps.append(host_inputs_C(np.ascontiguousarray(x1T[:, b * S:(b + 1) * S]), w_in1, f32(l1_conv_w), hq))
    outs = _run(ncC, maps)
    ymix1 = np.empty((4096, NTOK), dtype=ml_dtypes.bfloat16)
    for c in range(NCORES):
        b, hq = c // 4, c % 4
        ymix1[hq * 512:(hq + 1) * 512, b * S:(b + 1) * S] = outs[c]["ymix"][0:512]
        ymix1[2048 + hq * 512:2048 + (hq + 1) * 512, b * S:(b + 1) * S] = outs[c]["ymix"][512:1024]
    del outs, maps

    x2T = _layer_F(ymix1, x1T, 4096, f32(l1_w_out), f32(l1_ln_mix_g), f32(l1_ln_mix_b), f32(l1_ln_ffn_g), f32(l1_ln_ffn_b),
                   f32(l1_router_w), f32(l1_router_b), f32(l1_w1), f32(l1_b1), f32(l1_w2), f32(l1_b2))
    return np.ascontiguousarray(x2T.T).reshape(B, S, D).astype(np.float32)


def kernel(**inputs):
    return kernel_unfused(**inputs)
```
